# Optimizing a Trainium2 kernel written in Bass

```python
import math
import jax
import jax.numpy as jnp
from jax import lax
import numpy as np

D_MODEL = 1024
BATCH = 4
SEQ = 8192
DEPTH = 1

GRID_W = 64
CTX_LEN = 256
HEAD_DIM = 64
N_HEADS_DIFF = 8
N_HEADS_NA = 8
DIFF_QK_WIDTH = N_HEADS_DIFF * 2 * HEAD_DIM
DIFF_V_WIDTH = N_HEADS_DIFF * 2 * HEAD_DIM
NA_WIDTH = N_HEADS_NA * HEAD_DIM
IN_WIDTH = 2 * DIFF_QK_WIDTH + DIFF_V_WIDTH + 3 * NA_WIDTH + 2 * D_MODEL
NA_KH = 8
NA_KW = 16
Q_BLOCK = 128
ROPE_BASE = 10000.0
N_GROUPS = 4
EXPERTS_PER_GROUP = 8
N_EXPERTS = N_GROUPS * EXPERTS_PER_GROUP
TOP_K_IN_GROUP = 2
EXPERT_HIDDEN = 512
NORM_EPS = 1e-6

kernel_name = "hybrid_diffattn_natten_hmoe_block"


def rms_norm(x, w):
    xf = x.astype(jnp.float32)
    y = xf * lax.rsqrt(jnp.mean(xf * xf, axis=-1, keepdims=True) + NORM_EPS)
    return (y * w.astype(jnp.float32)).astype(x.dtype)


def adaln(cond, w_ada, b_ada):
    m = jax.nn.silu(cond) @ w_ada + b_ada
    return jnp.split(m, 6, axis=-1)


def modulate(h, shift, scale):
    return h * (1 + scale) + shift


def split_columns(p):
    widths = (DIFF_QK_WIDTH, DIFF_QK_WIDTH, DIFF_V_WIDTH, NA_WIDTH, NA_WIDTH, NA_WIDTH, D_MODEL, D_MODEL)
    points = [int(i) for i in np.cumsum(widths)[:-1]]
    return jnp.split(p, points, axis=-1)


def axial_rope_tables(n_tokens):
    t = jnp.arange(n_tokens, dtype=jnp.int32)
    pos = jnp.stack([t // GRID_W, t % GRID_W], axis=-1).astype(jnp.float32)
    n_freq = HEAD_DIM // 4
    inv_freq = ROPE_BASE ** (-jnp.arange(n_freq, dtype=jnp.float32) / n_freq)
    ang = pos[:, :, None] * inv_freq
    return jnp.cos(ang), jnp.sin(ang)


def apply_axial_rope(x, cos, sin):
    xr = x.reshape(*x.shape[:-1], 2, 2, HEAD_DIM // 4)
    x1, x2 = xr[..., 0, :], xr[..., 1, :]
    cs = cos[None, :, None].astype(x.dtype)
    sn = sin[None, :, None].astype(x.dtype)
    out = jnp.stack([x1 * cs - x2 * sn, x1 * sn + x2 * cs], axis=-2)
    return out.reshape(x.shape)


def diff_heads(q, k, v, q_norm, k_norm):
    b, l, _ = q.shape
    q = rms_norm(q.reshape(b, l, N_HEADS_DIFF, 2, HEAD_DIM), q_norm)
    k = rms_norm(k.reshape(b, l, N_HEADS_DIFF, 2, HEAD_DIM), k_norm)
    v = v.reshape(b, l, N_HEADS_DIFF, 2 * HEAD_DIM)
    return q[:, :, :, 0], q[:, :, :, 1], k[:, :, :, 0], k[:, :, :, 1], v


def diff_attend(q1, q2, k1, k2, v, lam):
    scale = HEAD_DIM ** -0.5
    p1 = jax.nn.softmax(jnp.einsum('bqhd,bkhd->bhqk', q1, k1).astype(jnp.float32) * scale, axis=-1)
    p2 = jax.nn.softmax(jnp.einsum('bqhd,bkhd->bhqk', q2, k2).astype(jnp.float32) * scale, axis=-1)
    p = (p1 - lam * p2).astype(v.dtype)
    return jnp.einsum('bhqk,bkhe->bqhe', p, v)


def diff_attention_blocks(q1, q2, k1, k2, v, lam):
    b, n, h, d = q1.shape
    nb = n // Q_BLOCK

    def blocks(q):
        return q.reshape(b, nb, Q_BLOCK, h, d).transpose(1, 0, 2, 3, 4)

    o = lax.map(lambda qs: diff_attend(qs[0], qs[1], k1, k2, v, lam), (blocks(q1), blocks(q2)))
    return o.transpose(1, 0, 2, 3, 4).reshape(b, n, h, 2 * d)


def softmax_attend(q, k, v):
    scale = HEAD_DIM ** -0.5
    p = jax.nn.softmax(jnp.einsum('bqhd,bkhd->bhqk', q, k).astype(jnp.float32) * scale, axis=-1)
    return jnp.einsum('bhqk,bkhd->bqhd', p.astype(v.dtype), v)


def neighbourhood_attention(q, k, v, k_ctx, v_ctx, rel_bias, rows):
    b, n, h, d = q.shape
    kh = min(NA_KH, rows)
    kw = NA_KW
    n_loc = kh * kw
    scale = d ** -0.5
    qg = q.reshape(b, rows, GRID_W, h, d)
    kg = k.reshape(b, rows, GRID_W, h, d)
    vg = v.reshape(b, rows, GRID_W, h, d)
    cols = np.arange(GRID_W)
    col_start = np.clip(cols - kw // 2, 0, GRID_W - kw)
    col_idx = col_start[:, None] + np.arange(kw)[None, :]
    col_bias_idx = col_idx - cols[:, None] + (NA_KW - 1)

    def row_block(r):
        r0 = jnp.clip(r - kh // 2, 0, rows - kh)
        q_row = lax.dynamic_index_in_dim(qg, r, axis=1, keepdims=False)
        k_rows = lax.dynamic_slice_in_dim(kg, r0, kh, axis=1)
        v_rows = lax.dynamic_slice_in_dim(vg, r0, kh, axis=1)
        k_win = k_rows[:, :, col_idx]
        v_win = v_rows[:, :, col_idx]
        row_bias_idx = r0 + jnp.arange(kh) - r + (NA_KH - 1)
        bias = rel_bias[:, row_bias_idx][:, :, col_bias_idx]
        s_loc = (jnp.einsum('bwhd,biwjhd->bhwij', q_row, k_win).astype(jnp.float32) * scale
                 + bias.transpose(0, 2, 1, 3)[None].astype(jnp.float32))
        s_ctx = jnp.einsum('bwhd,bchd->bhwc', q_row, k_ctx).astype(jnp.float32) * scale
        s = jnp.concatenate([s_loc.reshape(b, h, GRID_W, n_loc), s_ctx], axis=-1)
        p = jax.nn.softmax(s, axis=-1).astype(v.dtype)
        p_loc = p[..., :n_loc].reshape(b, h, GRID_W, kh, kw)
        p_ctx = p[..., n_loc:]
        return (jnp.einsum('bhwij,biwjhd->bwhd', p_loc, v_win)
                + jnp.einsum('bhwc,bchd->bwhd', p_ctx, v_ctx))

    o = lax.map(row_block, jnp.arange(rows, dtype=jnp.int32))
    return o.transpose(1, 0, 2, 3, 4).reshape(b, n, h * d)


def gated_merge(o_a, o_b, g_a, g_b, w_branch_a, w_branch_b, w_out):
    y = jax.nn.sigmoid(g_a) * (o_a @ w_branch_a) + jax.nn.sigmoid(g_b) * (o_b @ w_branch_b)
    return y @ w_out


def hierarchical_moe(h, w_rg, b_rg, w_re, b_re, w_gate, w_up, w_down):
    b, l, d = h.shape
    t = h.reshape(b * l, d)
    g_logits = (t @ w_rg + b_rg).astype(jnp.float32)
    g_prob = jax.nn.softmax(g_logits, axis=-1)
    g_idx = jnp.argmax(g_logits, axis=-1)
    g_w = jnp.max(g_prob, axis=-1, keepdims=True)
    e_logits = (jnp.einsum('td,gde->tge', t, w_re) + b_re).astype(jnp.float32)
    sel = jnp.einsum('tge,tg->te', e_logits, jax.nn.one_hot(g_idx, N_GROUPS, dtype=jnp.float32))
    top_vals, top_idx = lax.top_k(sel, TOP_K_IN_GROUP)
    weights = g_w * jax.nn.softmax(top_vals, axis=-1)
    expert_idx = g_idx[:, None] * EXPERTS_PER_GROUP + top_idx
    comb = jnp.einsum('tke,tk->te', jax.nn.one_hot(expert_idx, N_EXPERTS, dtype=jnp.float32), weights)
    comb = comb.astype(t.dtype)
    y = jnp.zeros_like(t)
    for e in range(N_EXPERTS):
        hid = jax.nn.silu(t @ w_gate[e]) * (t @ w_up[e])
        y = y + comb[:, e:e + 1] * (hid @ w_down[e])
    return y.reshape(b, l, d)


def setup_inputs(seed: int = 0) -> dict:
    key = jax.random.key(seed)
    ks = jax.random.split(key, 32)
    f32 = jnp.float32
    L, D = DEPTH, D_MODEL

    def nrm(k, shape, scale):
        return jax.random.normal(k, shape, f32) * scale

    def gain(k, shape):
        return 1.0 + 0.02 * jax.random.normal(k, shape, f32)

    return {
        "x": nrm(ks[0], (BATCH, SEQ, D), 1.0),
        "c": nrm(ks[1], (BATCH, D), 1.0),
        "ctx": nrm(ks[2], (BATCH, CTX_LEN, D), 1.0),
        "c_ctx": nrm(ks[3], (D,), 1.0),
        "w_ada": nrm(ks[4], (L, D, 6 * D), 0.5 * D ** -0.5),
        "b_ada": nrm(ks[5], (L, 6 * D), 0.02),
        "norm1_w": gain(ks[6], (L, D)),
        "norm2_w": gain(ks[7], (L, D)),
        "w_in": nrm(ks[8], (L, D, IN_WIDTH), D ** -0.5),
        "q_norm_a": gain(ks[9], (L, HEAD_DIM)),
        "k_norm_a": gain(ks[10], (L, HEAD_DIM)),
        "lambda_q1": nrm(ks[11], (L, HEAD_DIM), 0.1),
        "lambda_k1": nrm(ks[12], (L, HEAD_DIM), 0.1),
        "lambda_q2": nrm(ks[13], (L, HEAD_DIM), 0.1),
        "lambda_k2": nrm(ks[14], (L, HEAD_DIM), 0.1),
        "subln_a": gain(ks[15], (L, 2 * HEAD_DIM)),
        "q_norm_b": gain(ks[16], (L, HEAD_DIM)),
        "k_norm_b": gain(ks[17], (L, HEAD_DIM)),
        "na_rel_bias": nrm(ks[18], (L, N_HEADS_NA, 2 * NA_KH - 1, 2 * NA_KW - 1), 0.1),
        "w_branch_a": nrm(ks[19], (L, DIFF_V_WIDTH, D), DIFF_V_WIDTH ** -0.5),
        "w_branch_b": nrm(ks[20], (L, NA_WIDTH, D), NA_WIDTH ** -0.5),
        "w_out": nrm(ks[21], (L, D, D), D ** -0.5),
        "w_router_group": nrm(ks[22], (L, D, N_GROUPS), D ** -0.5),
        "b_router_group": nrm(ks[23], (L, N_GROUPS), 0.01),
        "w_router_expert": nrm(ks[24], (L, N_GROUPS, D, EXPERTS_PER_GROUP), D ** -0.5),
        "b_router_expert": nrm(ks[25], (L, N_GROUPS, EXPERTS_PER_GROUP), 0.01),
        "w_expert_gate": nrm(ks[26], (L, N_EXPERTS, D, EXPERT_HIDDEN), D ** -0.5),
        "w_expert_up": nrm(ks[27], (L, N_EXPERTS, D, EXPERT_HIDDEN), D ** -0.5),
        "w_expert_down": nrm(ks[28], (L, N_EXPERTS, EXPERT_HIDDEN, D), EXPERT_HIDDEN ** -0.5),
    }


def reference(x, c, ctx, c_ctx, w_ada, b_ada, norm1_w, norm2_w, w_in, q_norm_a, k_norm_a,
              lambda_q1, lambda_k1, lambda_q2, lambda_k2, subln_a, q_norm_b, k_norm_b, na_rel_bias,
              w_branch_a, w_branch_b, w_out, w_router_group, b_router_group, w_router_expert,
              b_router_expert, w_expert_gate, w_expert_up, w_expert_down):
    b, n, d = x.shape
    rows = n // GRID_W
    cos, sin = axial_rope_tables(n)
    for l in range(DEPTH):
        need_ctx = l < DEPTH - 1
        lam_init = 0.8 - 0.6 * math.exp(-0.3 * l)
        sh1, sc1, ga1, sh2, sc2, ga2 = adaln(c[:, None, :], w_ada[l], b_ada[l])
        csh1, csc1, cga1, csh2, csc2, cga2 = adaln(c_ctx[None, None, :], w_ada[l], b_ada[l])

        hx = modulate(rms_norm(x, norm1_w[l]), sh1, sc1)
        hc = modulate(rms_norm(ctx, norm1_w[l]), csh1, csc1)
        qa_x, ka_x, va_x, qb_x, kb_x, vb_x, gate_a_x, gate_b_x = split_columns(hx @ w_in[l])
        qa_c, ka_c, va_c, qb_c, kb_c, vb_c, gate_a_c, gate_b_c = split_columns(hc @ w_in[l])

        lam = (jnp.exp(jnp.sum(lambda_q1[l] * lambda_k1[l]).astype(jnp.float32))
               - jnp.exp(jnp.sum(lambda_q2[l] * lambda_k2[l]).astype(jnp.float32)) + lam_init)
        q1x, q2x, k1x, k2x, vax = diff_heads(qa_x, ka_x, va_x, q_norm_a[l], k_norm_a[l])
        q1c, q2c, k1c, k2c, vac = diff_heads(qa_c, ka_c, va_c, q_norm_a[l], k_norm_a[l])
        q1x, q2x = apply_axial_rope(q1x, cos, sin), apply_axial_rope(q2x, cos, sin)
        k1x, k2x = apply_axial_rope(k1x, cos, sin), apply_axial_rope(k2x, cos, sin)
        o_a = diff_attention_blocks(q1x, q2x,
                                    jnp.concatenate([k1c, k1x], axis=1),
                                    jnp.concatenate([k2c, k2x], axis=1),
                                    jnp.concatenate([vac, vax], axis=1), lam)
        o_a = (rms_norm(o_a, subln_a[l]) * (1 - lam_init)).reshape(b, n, DIFF_V_WIDTH)

        qbx = rms_norm(qb_x.reshape(b, n, N_HEADS_NA, HEAD_DIM), q_norm_b[l])
        kbx = rms_norm(kb_x.reshape(b, n, N_HEADS_NA, HEAD_DIM), k_norm_b[l])
        vbx = vb_x.reshape(b, n, N_HEADS_NA, HEAD_DIM)
        kbc = rms_norm(kb_c.reshape(b, CTX_LEN, N_HEADS_NA, HEAD_DIM), k_norm_b[l])
        vbc = vb_c.reshape(b, CTX_LEN, N_HEADS_NA, HEAD_DIM)
        o_b = neighbourhood_attention(qbx, kbx, vbx, kbc, vbc, na_rel_bias[l], rows)

        x_mid = x + ga1 * gated_merge(o_a, o_b, gate_a_x, gate_b_x, w_branch_a[l], w_branch_b[l], w_out[l])

        hx2 = modulate(rms_norm(x_mid, norm2_w[l]), sh2, sc2)
        x_new = x_mid + ga2 * hierarchical_moe(hx2, w_router_group[l], b_router_group[l], w_router_expert[l],
                                               b_router_expert[l], w_expert_gate[l], w_expert_up[l],
                                               w_expert_down[l])

        if need_ctx:
            o_a_c = diff_attend(q1c, q2c, k1c, k2c, vac, lam)
            o_a_c = (rms_norm(o_a_c, subln_a[l]) * (1 - lam_init)).reshape(b, CTX_LEN, DIFF_V_WIDTH)
            qbc = rms_norm(qb_c.reshape(b, CTX_LEN, N_HEADS_NA, HEAD_DIM), q_norm_b[l])
            o_b_c = softmax_attend(qbc, kbc, vbc).reshape(b, CTX_LEN, NA_WIDTH)
            ctx_mid = ctx + cga1 * gated_merge(o_a_c, o_b_c, gate_a_c, gate_b_c, w_branch_a[l], w_branch_b[l], w_out[l])
            hc2 = modulate(rms_norm(ctx_mid, norm2_w[l]), csh2, csc2)
            ctx = ctx_mid + cga2 * hierarchical_moe(hc2, w_router_group[l], b_router_group[l], w_router_expert[l],
                                                    b_router_expert[l], w_expert_gate[l], w_expert_up[l],
                                                    w_expert_down[l])
        x = x_new
    return x
```

```python
import contextlib
import numpy as np
import concourse.bass as bass
import concourse.mybir as mybir
from concourse.bass_utils import run_bass_kernel_spmd

F32 = mybir.dt.float32
BF16 = mybir.dt.bfloat16
I32 = mybir.dt.int32
AF = mybir.ActivationFunctionType
ALU = mybir.AluOpType
AX = mybir.AxisListType

D = 1024
NT = 8192
NOWN = 4096
NCTX = 256
NKA = NT + NCTX
NKB = 4608 + NCTX
INW = 6656
EPS = 1e-6
NEG = -30000.0
SEM_LIMIT = 8000
NSLOT = 96


class Buf:
    __slots__ = ("name", "lw", "rd")

    def __init__(self, name=""):
        self.name = name
        self.lw = None
        self.rd = []


class Prog:
    ENGS = ("sync", "act", "pool", "pe", "dve")

    def __init__(self, nc, stack):
        self.nc = nc
        self.stack = stack
        self.q = {e: [] for e in self.ENGS}
        self.cur = {}
        self.cnt = {}
        self.nsem = 0
        for e in ("act", "pool", "pe", "dve"):
            self._newsem(e)
        self.waited = {e: {} for e in self.ENGS}
        self.dma_pool = []
        self.dma_i = 0
        self.ninstr = 0

    def _mksem(self, name):
        self.nsem += 1
        return self.stack.enter_context(self.nc.semaphore(f"{name}_{self.nsem}"))

    def _newsem(self, e):
        self.cur[e] = self._mksem("s" + e)
        self.cnt[e] = 0

    def init_dma_pool(self, n, n_sw=8):
        for _ in range(n):
            self.dma_pool.append([self._mksem("dma"), 0])
        self.sw_pool = [[self._mksem("swdma"), 0] for _ in range(n_sw)]
        self.sw_i = 0

    def _dma_slot(self, eng):
        if eng == "pool":
            slot = self.sw_pool[self.sw_i % len(self.sw_pool)]
            self.sw_i += 1
        else:
            slot = self.dma_pool[self.dma_i % len(self.dma_pool)]
            self.dma_i += 1
        return slot

    def _wait(self, eng, h):
        if h is None:
            return
        sem, val, _ = h
        w = self.waited[eng]
        k = id(sem)
        if w.get(k, (None, 0))[1] >= val:
            return
        w[k] = (sem, val)
        self.q[eng].append(lambda e, sem=sem, val=val: e.wait_ge(sem, val))
        self.ninstr += 1

    def _deps(self, eng, reads, writes, extra):
        for b in reads:
            self._wait(eng, b.lw)
        for b in writes:
            if b.lw is not None and (b.lw[2] != eng or eng != "pe"):
                self._wait(eng, b.lw)
            for h in b.rd:
                if h[2] != eng or eng != "pe":
                    self._wait(eng, h)
        for h in extra:
            self._wait(eng, h)

    def _commit(self, h, reads, writes):
        for b in writes:
            b.lw = h
            b.rd = []
        for b in reads:
            b.rd.append(h)
            if len(b.rd) > 32:
                b.rd = b.rd[-32:]

    def op(self, eng, fn, reads=(), writes=(), extra=(), inc=True):
        self._deps(eng, reads, writes, extra)
        self.ninstr += 1
        if not inc:
            self.q[eng].append(lambda e, fn=fn: fn(e))
            return None
        if self.cnt[eng] >= SEM_LIMIT:
            self._newsem(eng)
        self.cnt[eng] += 1
        sem = self.cur[eng]
        h = (sem, self.cnt[eng], eng)
        self.q[eng].append(lambda e, fn=fn, sem=sem: fn(e).then_inc(sem, 1))
        self._commit(h, reads, writes)
        return h

    def dma(self, eng, out, in_, reads=(), writes=(), extra=(), **kw):
        self._deps(eng, reads, writes, extra)
        slot = self._dma_slot(eng)
        if slot[1] > 0:
            self._wait(eng, (slot[0], slot[1], "dma"))
        if slot[1] >= SEM_LIMIT * 16:
            slot[0] = self._mksem("dma")
            slot[1] = 0
        sem = slot[0]
        slot[1] += 16
        h = (sem, slot[1], "dma")
        self.q[eng].append(lambda e, out=out, in_=in_, sem=sem, kw=kw:
                           e.dma_start(out=out, in_=in_, **kw).then_inc(sem, 16))
        self.ninstr += 1
        self._commit(h, reads, writes)
        return h

    def idma(self, fn, reads=(), writes=(), eng="pool"):
        self._deps(eng, reads, writes, ())
        slot = self._dma_slot(eng)
        if slot[1] > 0:
            self._wait(eng, (slot[0], slot[1], "dma"))
        if slot[1] >= SEM_LIMIT * 16:
            slot[0] = self._mksem("dma")
            slot[1] = 0
        sem = slot[0]
        slot[1] += 16
        h = (sem, slot[1], "dma")
        self.q[eng].append(lambda e, fn=fn, sem=sem: fn(e).then_inc(sem, 16))
        self.ninstr += 1
        self._commit(h, reads, writes)
        return h

    def wait_all_dma(self, eng="sync"):
        for slot in self.dma_pool + self.sw_pool:
            if slot[1] > 0:
                self._wait(eng, (slot[0], slot[1], "dma"))

    def flush(self):
        self.wait_all_dma("sync")
        with self.nc.Block() as block:
            m = {"sync": block.sync, "act": block.scalar, "pool": block.gpsimd,
                 "pe": block.tensor, "dve": block.vector}
            for e in self.ENGS:
                fns = self.q[e]
                if not fns:
                    continue

                def body(engine, fns=fns):
                    for f in fns:
                        f(engine)
                m[e](body)
        self.q = {e: [] for e in self.ENGS}


class Ctx:
    pass


def build(debug=(), upto=99, opts=None):
    opts = opts or {}
    nc = bass.Bass("TRN2", target_bir_lowering=False)
    K = Ctx()
    K.nc = nc

    def din(name, shape, dt=F32):
        return nc.dram_tensor(name, list(shape), dt, kind="ExternalInput").ap()

    def dscr(name, shape, dt):
        kind = "ExternalOutput" if name in debug else "Internal"
        return nc.dram_tensor(name, list(shape), dt, kind=kind).ap()

    I = {}
    I["xall"] = din("xall", [NKA, D])
    I["cc"] = din("cc", [2, D])
    I["ropec"] = din("ropec", [NKA, 64])
    I["ropes"] = din("ropes", [NKA, 64])
    I["w_ada"] = din("w_ada", [D, 6 * D])
    I["b_ada"] = din("b_ada", [1, 6 * D])
    I["norm1_w"] = din("norm1_w", [1, D])
    I["norm2_w"] = din("norm2_w", [1, D])
    I["w_in"] = din("w_in", [D, INW])
    for n in ("q_norm_a", "k_norm_a", "lambda_q1", "lambda_k1", "lambda_q2", "lambda_k2", "q_norm_b", "k_norm_b"):
        I[n] = din(n, [1, 64])
    I["subln_a"] = din("subln_a", [1, 128])
    I["nabias"] = din("nabias", [2, 8, 8, 128, 512])
    I["w_branch_a"] = din("w_branch_a", [D, D])
    I["w_branch_b"] = din("w_branch_b", [512, D])
    I["w_out"] = din("w_out", [D, D])
    I["w_router"] = din("w_router", [D, 36])
    I["b_router"] = din("b_router", [1, 36])
    I["w_expert_gate"] = din("w_expert_gate", [32, D, 512])
    I["w_expert_up"] = din("w_expert_up", [32, D, 512])
    I["w_expert_down"] = din("w_expert_down", [32, 512, D])
    out = nc.dram_tensor("out", [NOWN, D], F32, kind="ExternalOutput").ap()

    S = {}
    S["M"] = dscr("M", [2, 6 * D], F32)
    S["QTA"] = dscr("QTA", [8, 128, NOWN], BF16)
    S["KTA"] = dscr("KTA", [8, 128, NKA], BF16)
    S["VA"] = dscr("VA", [NKA, 1024], BF16)
    S["QTB"] = dscr("QTB", [4, 128, NOWN], BF16)
    S["KTB"] = dscr("KTB", [4, 128, NKB], BF16)
    S["VB"] = dscr("VB", [NKB, 512], BF16)
    S["GA"] = dscr("GA", [NOWN, 1024], BF16)
    S["GB"] = dscr("GB", [NOWN, 1024], BF16)
    S["OAT"] = dscr("OAT", [8, 128, NOWN], BF16)
    S["OBT"] = dscr("OBT", [4, 128, NOWN], BF16)
    S["XMID"] = dscr("XMID", [NOWN, D], F32)
    S["HX2"] = dscr("HX2", [NOWN, D], BF16)
    S["ROUT"] = dscr("ROUT", [NOWN, 66], F32)
    S["TOK"] = dscr("TOK", [NSLOT * 128, 1], I32)
    S["Y"] = dscr("Y", [NSLOT * 128, D], F32)
    S["WG16"] = dscr("WG16", [32 * 128, 8 * 512], BF16)
    S["WU16"] = dscr("WU16", [32 * 128, 8 * 512], BF16)
    S["WD16"] = dscr("WD16", [32 * 128, 4 * D], BF16)
    if "DBGI" in debug:
        S["DBGI"] = dscr("DBGI", [128, 64 + 3 * NSLOT], I32)
        S["DBGF"] = dscr("DBGF", [128, 64 + NSLOT], F32)
    K.I, K.S, K.out = I, S, out

    with contextlib.ExitStack() as gst:
        P = Prog(nc, gst)
        P.init_dma_pool(12)
        K.P = P
        K.ident = gst.enter_context(nc.sbuf_tensor("ident", [128, 128], F32))
        K.identb = gst.enter_context(nc.sbuf_tensor("identb", [128, 128], BF16))
        K.b_ident = Buf("ident")
        K.b_identb = Buf("identb")
        P.op("pool", lambda e: e.memset(K.ident[:], 0.0), writes=[K.b_ident])
        P.op("pool", lambda e: e.affine_select(out=K.ident[:], in_=K.ident[:], pattern=[[-1, 128]],
                                               compare_op=ALU.not_equal, fill=1.0, base=0, channel_multiplier=1),
             reads=[K.b_ident], writes=[K.b_ident])
        P.op("pool", lambda e: e.tensor_copy(K.identb[:], K.ident[:]), reads=[K.b_ident], writes=[K.b_identb])

        phase0(K)
        P.flush()
        if upto >= 1:
            phase1(K)
            P.flush()
        if upto >= 2:
            phase2(K, **opts.get('p2', {}))
            P.flush()
        if upto >= 3:
            phase3(K, **opts.get('p3', {}))
            P.flush()
        if upto >= 4:
            phase4a(K, **opts.get('p4a', {}))
            P.flush()
        if upto >= 5:
            phase4s(K, **opts.get('p4s', {}))
            P.flush()
        K.ninstr = P.ninstr
    return nc, K


def phase0(K):
    nc, P, I, S = K.nc, K.P, K.I, K.S
    with contextlib.ExitStack() as st:
        sb = lambda n, s, d: st.enter_context(nc.sbuf_tensor(n, s, d))
        cc = sb("p0_cc", [2, D], F32)
        ccs = sb("p0_ccs", [2, D], F32)
        ccT = sb("p0_ccT", [128, 8, 2], F32)
        msb = sb("p0_m", [2, 6 * D], F32)
        bad = sb("p0_bad", [2, 6 * D], F32)
        wa = [sb(f"p0_wa{i}", [128, 8, 512], F32) for i in range(2)]
        pT = st.enter_context(nc.psum_tensor("p0_pT", [128, 8, 2], F32))
        pm = [st.enter_context(nc.psum_tensor(f"p0_pm{i}", [2, 512], F32)) for i in range(2)]
        b_cc, b_ccs, b_ccT, b_m, b_bad, b_pT = [Buf() for _ in range(6)]
        b_wa = [[Buf() for _ in range(8)] for _ in range(2)]
        b_pm = [Buf(), Buf()]
        P.dma("sync", cc[:], I["cc"], writes=[b_cc])
        P.dma("sync", bad[:], I["b_ada"].partition_broadcast(2), writes=[b_bad])
        P.op("act", lambda e: e.activation(out=ccs[:], in_=cc[:], func=AF.Silu), reads=[b_cc], writes=[b_ccs])
        for kc in range(8):
            P.op("pe", lambda e, kc=kc: e.transpose(pT[:, kc, :], ccs[:, kc * 128:(kc + 1) * 128], K.ident[0:2, 0:2]),
                 reads=[b_ccs, K.b_ident], writes=[b_pT])
        P.op("dve", lambda e: e.tensor_copy(ccT[:], pT[:]), reads=[b_pT], writes=[b_ccT])
        wav = I["w_ada"].rearrange("(kc p) n -> p kc n", p=128)
        for c in range(12):
            w = wa[c % 2]
            for kc in range(8):
                P.dma("sync", w[:, kc, :], wav[:, kc, c * 512:(c + 1) * 512], writes=[b_wa[c % 2][kc]])
            for kc in range(8):
                P.op("pe", lambda e, kc=kc, w=w, c=c: e.matmul(pm[c % 2][:], lhsT=ccT[:, kc, :], rhs=w[:, kc, :],
                                                              start=(kc == 0), stop=(kc == 7)),
                     reads=[b_ccT, b_wa[c % 2][kc]], writes=[b_pm[c % 2]])
            P.op("dve", lambda e, c=c: e.tensor_tensor(out=msb[:, c * 512:(c + 1) * 512], in0=pm[c % 2][:],
                                                       in1=bad[:, c * 512:(c + 1) * 512], op=ALU.add),
                 reads=[b_pm[c % 2], b_bad], writes=[b_m])
        P.dma("sync", S["M"], msb[:], reads=[b_m])
        P.wait_all_dma("sync")


def load_bcast(K, P, tile, dram_row, buf):
    return P.dma("sync", tile, dram_row.partition_broadcast(128), writes=[buf])


def phase1(K):
    nc, P, I, S = K.nc, K.P, K.I, K.S
    with contextlib.ExitStack() as st:
        sb = lambda n, s, d: st.enter_context(nc.sbuf_tensor(n, s, d))
        ps = lambda n, s, d: st.enter_context(nc.psum_tensor(n, s, d))
        Wb = sb("p1_W", [128, 8, INW], BF16)
        b_W = [[Buf() for _ in range(8)] for _ in range(13)]
        winv = I["w_in"].rearrange("(kc p) n -> p kc n", p=128)
        C1 = [2, 3, 4, 5, 7, 8]
        C2 = [0, 1, 6, 9, 10, 11, 12]

        def load_w(c):
            for kc in range(8):
                P.dma("pool", Wb[:, kc, c * 512:(c + 1) * 512], winv[:, kc, c * 512:(c + 1) * 512], writes=[b_W[c][kc]])

        g1 = [sb(f"p1_g1_{i}", [128, D], F32) for i in range(2)]
        sh = [sb(f"p1_sh_{i}", [128, D], F32) for i in range(2)]
        b_g1 = [Buf(), Buf()]
        b_sh = [Buf(), Buf()]
        hx = [sb(f"p1_hx{i}", [128, D], F32) for i in range(2)]
        b_hx = [Buf(), Buf()]
        n1 = hx[0]
        b_n1 = b_hx[0]
        load_bcast(K, P, n1[:], I["norm1_w"], b_n1)
        for r in range(2):
            load_bcast(K, P, g1[r][:], S["M"][r:r + 1, 1024:2048], b_g1[r])
            load_bcast(K, P, sh[r][:], S["M"][r:r + 1, 0:1024], b_sh[r])
            P.op("dve", lambda e, r=r: e.scalar_tensor_tensor(out=g1[r][:], in0=g1[r][:], scalar=1.0, in1=n1[:],
                                                             op0=ALU.add, op1=ALU.mult),
                 reads=[b_g1[r], b_n1], writes=[b_g1[r]])
        gains = {}
        b_gain = Buf()
        for n in ("q_norm_a", "k_norm_a", "q_norm_b", "k_norm_b"):
            gains[n] = sb("p1_" + n, [128, 64], F32)
            load_bcast(K, P, gains[n][:], I[n], b_gain)
        nhalf = sb("p1_nhalf", [128, 24], F32)
        b_nhalf = Buf()
        P.op("pool", lambda e: e.memset(nhalf[:], -0.5), writes=[b_nhalf])
        for c in C1:
            load_w(c)
        pending_w = list(C2)

        NXB, NRB = 3, 6
        xt = [sb(f"p1_xt{i}", [128, D], F32) for i in range(NXB)]
        b_xt = [Buf() for _ in range(NXB)]
        rc = [sb(f"p1_rc{i}", [128, 64], F32) for i in range(NRB)]
        rs_ = [sb(f"p1_rs{i}", [128, 64], F32) for i in range(NRB)]
        b_rope = [Buf() for _ in range(NRB)]
        junk = sb("p1_junk", [128, D], BF16)
        b_junk = Buf()
        ss = [sb(f"p1_ss{i}", [128, 1], F32) for i in range(2)]
        b_ss = [Buf(), Buf()]
        rstd = [sb(f"p1_rstd{i}", [128, 1], F32) for i in range(2)]
        b_rstd = [Buf(), Buf()]
        pT = ps("p1_pT", [128, 8, 128], F32)
        b_pT = Buf()
        hxT = [sb(f"p1_hxT{i}", [128, 8, 128], BF16) for i in range(2)]
        b_hxT = [Buf(), Buf()]
        NPC = 4
        pc = [ps(f"p1_pc{i}", [128, 512], F32) for i in range(NPC)]
        b_pc = [Buf() for _ in range(NPC)]
        pTb = [ps(f"p1_pTb{i}", [128, 8, 128], BF16) for i in range(2)]
        b_pTb = [Buf(), Buf()]
        T = [sb(f"p1_T{i}", [128, 24, 64], F32) for i in range(2)]
        b_T = [Buf(), Buf()]
        sq = sb("p1_sq", [128, 24, 64], F32)
        b_sq = Buf()
        ss8 = [sb(f"p1_ss8{i}", [128, 24], F32) for i in range(2)]
        b_ss8 = [Buf(), Buf()]
        r8 = [sb(f"p1_r8{i}", [128, 24], F32) for i in range(2)]
        b_r8 = [Buf(), Buf()]
        ra = sb("p1_ra", [128, 16, 64], F32)
        b_ra = Buf()
        rb = sb("p1_rb", [128, 16, 64], F32)
        b_rb = Buf()
        tn = [sb(f"p1_tn{i}", [128, 24 * 64], BF16) for i in range(2)]
        b_tn = [Buf(), Buf()]
        stA = [sb(f"p1_stA{i}", [128, 8, 256], BF16) for i in range(2)]
        stB = [sb(f"p1_stB{i}", [128, 4, 256], BF16) for i in range(2)]
        b_stA, b_stB = [Buf(), Buf()], [Buf(), Buf()]
        vg = [sb(f"p1_vg{i}", [128, 2048], BF16) for i in range(2)]
        b_vg = [Buf(), Buf()]

        seq = []
        for t in range(32):
            seq.append((1, t * 128, "A"))
        for t in range(32, 64):
            seq.append((1, t * 128, "B1" if t < 36 else "B"))
        for t in range(64, 66):
            seq.append((1, t * 128, "C"))
        for t in range(32):
            seq.append((2, t * 128, "Q"))
        N = len(seq)
        chunks_of = {"A": C1, "B1": C1, "C": C1, "B": [2, 3, 4, 5], "Q": C2}
        SLOT0 = {2: 0, 3: 8, 7: 16, 0: 0, 1: 8, 6: 16}
        cnt = {"pc": 0, "pTb": 0}

        def issue_load(i):
            if i >= N:
                return
            _, u0, _ = seq[i]
            j, jr = i % NXB, i % NRB
            P.dma("sync", xt[j][:], I["xall"][u0:u0 + 128, :], writes=[b_xt[j]])
            P.dma("sync", rc[jr][:], I["ropec"][u0:u0 + 128, :], writes=[b_rope[jr]])
            P.dma("sync", rs_[jr][:], I["ropes"][u0:u0 + 128, :], writes=[b_rope[jr]])

        def S1a(i):
            if i >= N:
                return
            _, u0, kind = seq[i]
            j, d = i % NXB, i % 2
            m = 1 if kind == "C" else 0
            P.op("act", lambda e: e.activation(out=junk[:], in_=xt[j][:], func=AF.Square, accum_out=ss[d][:]),
                 reads=[b_xt[j]], writes=[b_junk, b_ss[d]])
            P.op("dve", lambda e: e.tensor_scalar(out=ss[d][:], in0=ss[d][:], scalar1=1.0 / D, scalar2=EPS,
                                                  op0=ALU.mult, op1=ALU.add), reads=[b_ss[d]], writes=[b_ss[d]])
            P.op("pool", lambda e: e.tensor_tensor(out=rstd[d][:], in0=ss[d][:], in1=nhalf[:, 0:1], op=ALU.pow),
                 reads=[b_ss[d], b_nhalf], writes=[b_rstd[d]])
            P.op("dve", lambda e: e.scalar_tensor_tensor(out=hx[d][:], in0=xt[j][:], scalar=rstd[d][:, 0:1],
                                                         in1=g1[m][:], op0=ALU.mult, op1=ALU.mult),
                 reads=[b_xt[j], b_rstd[d], b_g1[m]], writes=[b_hx[d]])
            P.op("pool", lambda e: e.tensor_tensor(out=hx[d][:], in0=hx[d][:], in1=sh[m][:], op=ALU.add),
                 reads=[b_hx[d], b_sh[m]], writes=[b_hx[d]])

        def S1b(i):
            if i >= N:
                return
            d = i % 2
            for kc in range(8):
                P.op("pe", lambda e, kc=kc: e.transpose(pT[:, kc, :], hx[d][:, kc * 128:(kc + 1) * 128], K.ident[:]),
                     reads=[b_hx[d], K.b_ident], writes=[b_pT], inc=(kc == 7))
            P.op("act", lambda e: e.copy(out=hxT[d][:], in_=pT[:]), reads=[b_pT], writes=[b_hxT[d]])

        def S2(i):
            pas, u0, kind = seq[i]
            d = i % 2
            for c in chunks_of[kind]:
                pi = cnt["pc"] % NPC
                cnt["pc"] += 1
                for kc in range(8):
                    P.op("pe", lambda e, kc=kc, c=c, pi=pi: e.matmul(pc[pi][:], lhsT=hxT[d][:, kc, :],
                                                                    rhs=Wb[:, kc, c * 512:(c + 1) * 512],
                                                                    start=(kc == 0), stop=(kc == 7)),
                         reads=[b_hxT[d], b_W[c][kc]], writes=[b_pc[pi]], inc=(kc == 7))
                if c in SLOT0:
                    s0 = SLOT0[c]
                    P.op("act", lambda e, pi=pi, s0=s0: e.copy(out=T[d][:, s0:s0 + 8, :].rearrange("p a b -> p (a b)"), in_=pc[pi][:]),
                         reads=[b_pc[pi]], writes=[b_T[d]])
                elif c in (4, 5, 8):
                    off = {4: 0, 5: 512, 8: 1024}[c]
                    P.op("act", lambda e, pi=pi, off=off: e.copy(out=vg[d][:, off:off + 512], in_=pc[pi][:]),
                         reads=[b_pc[pi]], writes=[b_vg[d]])
                else:
                    off = (c - 9) * 512
                    P.op("act", lambda e, pi=pi, off=off: e.activation(out=vg[d][:, off:off + 512], in_=pc[pi][:], func=AF.Sigmoid),
                         reads=[b_pc[pi]], writes=[b_vg[d]])
            if pas == 1:
                P.dma("sync", S["VA"][u0:u0 + 128, :], vg[d][:, 0:1024], reads=[b_vg[d]])
                if kind != "B":
                    ub = u0 if kind != "C" else 4608 + (u0 - NT)
                    P.dma("sync", S["VB"][ub:ub + 128, :], vg[d][:, 1024:1536], reads=[b_vg[d]])
            else:
                P.dma("sync", S["GA"][u0:u0 + 128, :], vg[d][:, 0:1024], reads=[b_vg[d]])
                P.dma("sync", S["GB"][u0:u0 + 128, :], vg[d][:, 1024:2048], reads=[b_vg[d]])

        def S3(i):
            pas, u0, kind = seq[i]
            d, jr = i % 2, i % NRB
            n = 16 if kind == "B" else 24
            gA = gains["k_norm_a" if pas == 1 else "q_norm_a"]
            gB = gains["k_norm_b" if pas == 1 else "q_norm_b"]
            Tt = T[d]
            tn3 = tn[d][:].rearrange("p (a b) -> p a b", b=64)
            P.op("dve", lambda e: e.tensor_tensor(out=sq[:, 0:n, :], in0=Tt[:, 0:n, :], in1=Tt[:, 0:n, :], op=ALU.mult),
                 reads=[b_T[d]], writes=[b_sq])
            P.op("dve", lambda e: e.tensor_reduce(out=ss8[d][:, 0:n], in_=sq[:, 0:n, :], axis=AX.X, op=ALU.add),
                 reads=[b_sq], writes=[b_ss8[d]])
            P.op("dve", lambda e: e.tensor_scalar(out=ss8[d][:, 0:n], in0=ss8[d][:, 0:n], scalar1=1.0 / 64, scalar2=EPS,
                                                  op0=ALU.mult, op1=ALU.add), reads=[b_ss8[d]], writes=[b_ss8[d]])
            P.op("pool", lambda e: e.tensor_tensor(out=r8[d][:, 0:n], in0=ss8[d][:, 0:n], in1=nhalf[:, 0:n], op=ALU.pow),
                 reads=[b_ss8[d], b_nhalf], writes=[b_r8[d]])
            P.op("dve", lambda e: e.tensor_tensor(out=Tt[:, 0:n, :], in0=Tt[:, 0:n, :],
                                                  in1=r8[d][:, 0:n].unsqueeze(2).to_broadcast([128, n, 64]), op=ALU.mult),
                 reads=[b_T[d], b_r8[d]], writes=[b_T[d]])
            P.op("dve", lambda e: e.tensor_tensor(out=Tt[:, 0:16, :], in0=Tt[:, 0:16, :],
                                                  in1=gA[:].unsqueeze(1).to_broadcast([128, 16, 64]), op=ALU.mult),
                 reads=[b_T[d], b_gain], writes=[b_T[d]])
            if n == 24:
                P.op("dve", lambda e: e.tensor_tensor(out=tn3[:, 16:24, :], in0=Tt[:, 16:24, :],
                                                      in1=gB[:].unsqueeze(1).to_broadcast([128, 8, 64]), op=ALU.mult),
                     reads=[b_T[d], b_gain], writes=[b_tn[d]])
            P.op("dve", lambda e: e.tensor_tensor(out=ra[:], in0=Tt[:, 0:16, :],
                                                  in1=rc[jr][:].unsqueeze(1).to_broadcast([128, 16, 64]), op=ALU.mult),
                 reads=[b_T[d], b_rope[jr]], writes=[b_ra])
            t5 = Tt[:, 0:16, :].rearrange("p a (x y f) -> p a x y f", x=2, y=2)
            rb5 = rb[:].rearrange("p a (x y f) -> p a x y f", x=2, y=2)
            s5 = rs_[jr][:].rearrange("p (x y f) -> p x y f", x=2, y=2)
            for a in range(2):
                for y in range(2):
                    P.op("pool", lambda e, a=a, y=y: e.tensor_tensor(
                        out=rb5[:, :, a, y, :], in0=t5[:, :, a, 1 - y, :],
                        in1=s5[:, a, y, :].unsqueeze(1).to_broadcast([128, 16, 16]), op=ALU.mult),
                         reads=[b_T[d], b_rope[jr]], writes=[b_rb])
            P.op("dve", lambda e: e.tensor_tensor(out=tn3[:, 0:16, :], in0=ra[:], in1=rb[:], op=ALU.add),
                 reads=[b_ra, b_rb], writes=[b_tn[d]])

        def S4(i):
            if i < 0:
                return
            pas, u0, kind = seq[i]
            d, gi, tg = i % 2, (i // 2) % 2, i % 2
            qi = cnt["pTb"] % 2
            cnt["pTb"] += 1
            for jj in range(8):
                P.op("pe", lambda e, jj=jj: e.transpose(pTb[qi][:, jj, :], tn[d][:, jj * 128:(jj + 1) * 128], K.identb[:]),
                     reads=[b_tn[d], K.b_identb], writes=[b_pTb[qi]], inc=(jj == 7))
            P.op("act", lambda e: e.copy(out=stA[gi][:, :, tg * 128:(tg + 1) * 128], in_=pTb[qi][:]),
                 reads=[b_pTb[qi]], writes=[b_stA[gi]])
            if kind != "B":
                q2 = cnt["pTb"] % 2
                cnt["pTb"] += 1
                for jj in range(4):
                    P.op("pe", lambda e, jj=jj: e.transpose(pTb[q2][:, jj, :], tn[d][:, (8 + jj) * 128:(9 + jj) * 128], K.identb[:]),
                         reads=[b_tn[d], K.b_identb], writes=[b_pTb[q2]], inc=(jj == 3))
                P.op("act", lambda e: e.copy(out=stB[gi][:, :, tg * 128:(tg + 1) * 128], in_=pTb[q2][:, 0:4, :]),
                     reads=[b_pTb[q2]], writes=[b_stB[gi]])
            if tg == 1:
                g0 = u0 - 128
                if pas == 1:
                    P.dma("sync", S["KTA"][:, :, g0:g0 + 256].rearrange("h p t -> p h t"), stA[gi][:], reads=[b_stA[gi]])
                    if kind != "B":
                        gb0 = g0 if kind != "C" else 4608 + (g0 - NT)
                        P.dma("sync", S["KTB"][:, :, gb0:gb0 + 256].rearrange("h p t -> p h t"), stB[gi][:], reads=[b_stB[gi]])
                else:
                    P.dma("sync", S["QTA"][:, :, g0:g0 + 256].rearrange("h p t -> p h t"), stA[gi][:], reads=[b_stA[gi]])
                    P.dma("sync", S["QTB"][:, :, g0:g0 + 256].rearrange("h p t -> p h t"), stB[gi][:], reads=[b_stB[gi]])

        issue_load(0)
        issue_load(1)
        issue_load(2)
        S1a(0)
        S1a(1)
        S1b(0)
        for i in range(N):
            issue_load(i + 3)
            if pending_w and i % 4 == 1:
                load_w(pending_w.pop(0))
            S4(i - 2)
            S1b(i + 1)
            S2(i)
            if i >= 1:
                S3(i - 1)
            S1a(i + 2)
        S3(N - 1)
        S4(N - 2)
        S4(N - 1)
        P.wait_all_dma("sync")


LAM_INIT = 0.2


def phase2(K, heads=range(8), qblocks=range(8), precast=True):
    nc, P, I, S = K.nc, K.P, K.I, K.S
    NKT = NKA // 128
    with contextlib.ExitStack() as st:
        sb = lambda n, s, d: st.enter_context(nc.sbuf_tensor(n, s, d))
        ps = lambda n, s, d: st.enter_context(nc.psum_tensor(n, s, d))
        KT = [sb(f"p2_KT{i}", [128, NKA], BF16) for i in range(2)]
        QT = [sb(f"p2_QT{i}", [128, NOWN], BF16) for i in range(2)]
        V = [sb(f"p2_V{i}", [128, NKT, 130], BF16) for i in range(2)]
        b_KT, b_QT, b_V = [Buf(), Buf()], [Buf(), Buf()], [Buf(), Buf()]
        for i in range(2):
            P.op("pool", lambda e, i=i: e.memset(V[i][:, :, 128:130], 1.0), writes=[b_V[i]])
        lv = sb("p2_lv", [128, 4, 64], F32)
        b_lv = Buf()
        for i, n in enumerate(("lambda_q1", "lambda_k1", "lambda_q2", "lambda_k2")):
            load_bcast(K, P, lv[:, i, :], I[n], b_lv)
        lprod = sb("p2_lprod", [128, 2, 64], F32)
        lsum = sb("p2_lsum", [128, 2], F32)
        lam = sb("p2_lam", [128, 1], F32)
        b_lam = Buf()
        P.op("dve", lambda e: e.tensor_tensor(out=lprod[:, 0, :], in0=lv[:, 0, :], in1=lv[:, 1, :], op=ALU.mult), reads=[b_lv], writes=[b_lam])
        P.op("dve", lambda e: e.tensor_tensor(out=lprod[:, 1, :], in0=lv[:, 2, :], in1=lv[:, 3, :], op=ALU.mult), reads=[b_lv], writes=[b_lam])
        P.op("dve", lambda e: e.tensor_reduce(out=lsum[:], in_=lprod[:], axis=AX.X, op=ALU.add), reads=[b_lam], writes=[b_lam])
        P.op("act", lambda e: e.activation(out=lsum[:], in_=lsum[:], func=AF.Exp), reads=[b_lam], writes=[b_lam])
        P.op("dve", lambda e: e.tensor_tensor(out=lam[:], in0=lsum[:, 0:1], in1=lsum[:, 1:2], op=ALU.subtract), reads=[b_lam], writes=[b_lam])
        P.op("dve", lambda e: e.tensor_scalar(out=lam[:], in0=lam[:], scalar1=LAM_INIT, scalar2=None, op0=ALU.add), reads=[b_lam], writes=[b_lam])
        gsub = sb("p2_gsub", [128, 128], F32)
        b_gsub = Buf()
        load_bcast(K, P, gsub[:], I["subln_a"], b_gsub)
        P.op("dve", lambda e: e.tensor_scalar(out=gsub[:], in0=gsub[:], scalar1=1.0 - LAM_INIT, scalar2=None, op0=ALU.mult),
             reads=[b_gsub], writes=[b_gsub])
        nhalf = sb("p2_nhalf", [128, 1], F32)
        b_nhalf = Buf()
        P.op("pool", lambda e: e.memset(nhalf[:], -0.5), writes=[b_nhalf])

        pS = [ps(f"p2_pS{i}", [128, 1024], F32) for i in range(2)]
        b_pS = [Buf(), Buf()]
        pO = [ps(f"p2_pO{i}", [128, 512], F32) for i in range(3)]
        b_pO = Buf()
        pTr = ps("p2_pTr", [128, 8, 128], BF16)
        b_pTr = Buf()
        NPT = 3
        pt = [sb(f"p2_pt{i}", [128, 1024], BF16) for i in range(NPT)]
        b_pt = [Buf() for _ in range(NPT)]
        def acc(j):
            return pO[j // 3][:, (j % 3) * 129:(j % 3) * 129 + 129]
        osb = [sb(f"p2_osb{i}", [128, 8, 129], F32) for i in range(2)]
        b_osb = [Buf(), Buf()]
        rz = sb("p2_rz", [128, 8], F32)
        b_rz = Buf()
        o1s = sb("p2_o1s", [128, 4, 128], F32)
        o2s = sb("p2_o2s", [128, 4, 128], F32)
        osq = sb("p2_osq", [128, 4, 128], F32)
        b_f = Buf()
        oss = sb("p2_oss", [128, 4], F32)
        ors = sb("p2_ors", [128, 4], F32)
        b_oss, b_ors = Buf(), Buf()
        nhalf4 = sb("p2_nhalf4", [128, 4], F32)
        P.op("pool", lambda e: e.memset(nhalf4[:], -0.5), writes=[b_nhalf])
        onb = [sb(f"p2_on{i}", [128, 4, 128], BF16) for i in range(2)]
        b_on = [Buf(), Buf()]
        oT = [sb(f"p2_oT{i}", [128, 512], BF16) for i in range(2)]
        b_oT = [Buf(), Buf()]

        def load_head(h, par):
            P.dma("sync", KT[par][:], S["KTA"][h], writes=[b_KT[par]])
            P.dma("sync", QT[par][:], S["QTA"][h], writes=[b_QT[par]])
            vv = S["VA"][:, h * 128:(h + 1) * 128].rearrange("(kt p) d -> p kt d", p=128)
            for k0 in range(0, NKT, 22):
                P.dma("sync", V[par][:, k0:k0 + 22, 0:128], vv[:, k0:k0 + 22, :], writes=[b_V[par]])

        heads = list(heads)
        qblocks = list(qblocks)
        its = [(hi, h, qb, kt) for hi, h in enumerate(heads) for qb in qblocks for kt in range(NKT)]

        def emit_S(n):
            if n >= len(its):
                return
            hi, h, qb, kt = its[n]
            par = hi % 2
            sp = n % 2
            P.op("pe", lambda e: e.matmul(pS[sp][:, 0:512], lhsT=KT[par][0:64, kt * 128:(kt + 1) * 128],
                                          rhs=QT[par][0:64, qb * 512:(qb + 1) * 512], start=True, stop=True, tile_position=(0, 0)),
                 reads=[b_KT[par], b_QT[par]], writes=[b_pS[sp]], inc=False)
            P.op("pe", lambda e: e.matmul(pS[sp][:, 512:1024], lhsT=KT[par][64:128, kt * 128:(kt + 1) * 128],
                                          rhs=QT[par][64:128, qb * 512:(qb + 1) * 512], start=True, stop=True, tile_position=(64, 0)),
                 reads=[b_KT[par], b_QT[par]], writes=[b_pS[sp]])

        def fin_part1(oi):
            ob3 = osb[oi]
            P.op("dve", lambda e: e.tensor_copy(ob3[:, 0:3, :], pO[0][:, 0:387].rearrange("p (a b) -> p a b", b=129)), reads=[b_pO], writes=[b_osb[oi]])
            P.op("dve", lambda e: e.tensor_copy(ob3[:, 3:6, :], pO[1][:, 0:387].rearrange("p (a b) -> p a b", b=129)), reads=[b_pO], writes=[b_osb[oi]])
            P.op("dve", lambda e: e.tensor_copy(ob3[:, 6:8, :], pO[2][:, 0:258].rearrange("p (a b) -> p a b", b=129)), reads=[b_pO], writes=[b_osb[oi]])
            P.op("dve", lambda e: e.reciprocal(out=rz[:].unsqueeze(2), in_=ob3[:, :, 128:129]), reads=[b_osb[oi]], writes=[b_rz])
            P.op("dve", lambda e: e.tensor_scalar(out=rz[:, 4:8], in0=rz[:, 4:8], scalar1=lam[:, 0:1], scalar2=None, op0=ALU.mult),
                 reads=[b_rz, b_lam], writes=[b_rz])
            P.op("dve", lambda e: e.tensor_tensor(out=o2s[:], in0=ob3[:, 4:8, 0:128], in1=rz[:, 4:8].unsqueeze(2).to_broadcast([128, 4, 128]), op=ALU.mult),
                 reads=[b_osb[oi], b_rz], writes=[b_f])
            P.op("dve", lambda e: e.tensor_tensor(out=o1s[:], in0=ob3[:, 0:4, 0:128], in1=rz[:, 0:4].unsqueeze(2).to_broadcast([128, 4, 128]), op=ALU.mult),
                 reads=[b_osb[oi], b_rz], writes=[b_f])
            P.op("dve", lambda e: e.tensor_tensor(out=o1s[:], in0=o1s[:], in1=o2s[:], op=ALU.subtract), reads=[b_f], writes=[b_f])
            P.op("dve", lambda e: e.tensor_tensor(out=osq[:], in0=o1s[:], in1=o1s[:], op=ALU.mult), reads=[b_f], writes=[b_f])
            P.op("dve", lambda e: e.tensor_reduce(out=oss[:], in_=osq[:], axis=AX.X, op=ALU.add), reads=[b_f], writes=[b_oss])
            P.op("dve", lambda e: e.tensor_scalar(out=oss[:], in0=oss[:], scalar1=1.0 / 128, scalar2=EPS, op0=ALU.mult, op1=ALU.add),
                 reads=[b_oss], writes=[b_oss])
            P.op("pool", lambda e: e.tensor_tensor(out=ors[:], in0=oss[:], in1=nhalf4[:], op=ALU.pow), reads=[b_oss, b_nhalf], writes=[b_ors])
            P.op("dve", lambda e: e.tensor_tensor(out=o1s[:], in0=o1s[:], in1=ors[:].unsqueeze(2).to_broadcast([128, 4, 128]), op=ALU.mult),
                 reads=[b_f, b_ors], writes=[b_f])
            P.op("dve", lambda e: e.tensor_tensor(out=onb[oi][:], in0=o1s[:], in1=gsub[:].unsqueeze(1).to_broadcast([128, 4, 128]), op=ALU.mult),
                 reads=[b_f, b_gsub], writes=[b_on[oi]])

        def fin_part2(h, qb, oi):
            for qi in range(4):
                P.op("pe", lambda e, qi=qi: e.transpose(pTr[:, qi, :], onb[oi][:, qi, :], K.identb[:]),
                     reads=[b_on[oi], K.b_identb], writes=[b_pTr], inc=(qi == 3))
            P.op("dve", lambda e: e.tensor_copy(oT[oi][:].rearrange("p (a b) -> p a b", b=128), pTr[:, 0:4, :]),
                 reads=[b_pTr], writes=[b_oT[oi]])
            P.dma("sync", S["OAT"][h][:, qb * 512:(qb + 1) * 512], oT[oi][:], reads=[b_oT[oi]])

        cst = {m: [sb(f"p2_cst_{m}{i}", [128, 4096], BF16) for i in range(2)] for m in "gud"}
        b_cst = {m: [[Buf() for _ in range(8)] for _ in range(2)] for m in "gud"}
        units = [(e, m) for e in range(32) for m in "gud"] if precast else []

        def emit_cast(u):
            e, m = units[u]
            par = e % 2
            if m == "d":
                v = I["w_expert_down"][e].rearrange("(c p) n -> p c n", p=128)
                t3 = cst[m][par][:].rearrange("p (a b) -> p a b", b=D)
                nch, dst = 4, S["WD16"]
            else:
                v = (I["w_expert_gate"] if m == "g" else I["w_expert_up"])[e].rearrange("(c p) n -> p c n", p=128)
                t3 = cst[m][par][:].rearrange("p (a b) -> p a b", b=512)
                nch, dst = 8, (S["WG16"] if m == "g" else S["WU16"])
            for c in range(nch):
                P.dma("pool", t3[:, c, :], v[:, c, :], writes=[b_cst[m][par][c]])
            P.dma("sync", dst[e * 128:(e + 1) * 128, :], cst[m][par][:], reads=b_cst[m][par][0:nch])

        load_head(heads[0], 0)
        emit_S(0)
        emit_S(1)
        fin = 0
        pending = None
        ucast = 0
        for n, (hi, h, qb, kt) in enumerate(its):
            par = hi % 2
            if qb == qblocks[0] and kt == 0 and hi + 1 < len(heads):
                load_head(heads[hi + 1], (hi + 1) % 2)
            sp = n % 2
            pi = n % NPT
            P.op("act", lambda e, sp=sp, pi=pi: e.activation(out=pt[pi][:], in_=pS[sp][:], func=AF.Exp, scale=0.125),
                 reads=[b_pS[sp]], writes=[b_pt[pi]])
            emit_S(n + 2)
            for j in range(8):
                half, qi = j // 4, j % 4
                P.op("pe", lambda e, j=j, half=half, qi=qi, pi=pi, par=par, kt=kt: e.matmul(
                    acc(j), lhsT=pt[pi][:, half * 512 + qi * 128: half * 512 + (qi + 1) * 128], rhs=V[par][:, kt, 0:129],
                    start=(kt == 0 and j % 3 == 0), stop=(kt == NKT - 1), skip_group_check=True),
                     reads=[b_pt[pi], b_V[par]], writes=[b_pO], inc=(j == 7))
            if kt == 20 and pending is not None:
                fin_part2(*pending)
                pending = None
            if n % 43 == 5 and ucast < len(units):
                emit_cast(ucast)
                ucast += 1
            if kt == NKT - 1:
                if pending is not None:
                    fin_part2(*pending)
                oi = fin % 2
                fin += 1
                fin_part1(oi)
                pending = (h, qb, oi)
        if pending is not None:
            fin_part2(*pending)
        while ucast < len(units):
            emit_cast(ucast)
            ucast += 1
        P.wait_all_dma("sync")


def phase3(K, pairs=range(4), slots=range(8)):
    nc, P, I, S = K.nc, K.P, K.I, K.S
    NKT = NKB // 128
    with contextlib.ExitStack() as st:
        sb = lambda n, s, d: st.enter_context(nc.sbuf_tensor(n, s, d))
        ps = lambda n, s, d: st.enter_context(nc.psum_tensor(n, s, d))
        KT2 = [sb(f"p3_KT{i}", [128, NKB], BF16) for i in range(2)]
        QT2 = [sb(f"p3_QT{i}", [128, NOWN], BF16) for i in range(2)]
        V2 = [sb(f"p3_V{i}", [128, NKT, 2, 66], BF16) for i in range(2)]
        b_KT2, b_QT2 = [Buf(), Buf()], [Buf(), Buf()]
        b_V2 = [[Buf(), Buf()] for _ in range(2)]
        for i in range(2):
            P.op("pool", lambda e, i=i: e.memset(V2[i][:, :, :, 64:66], 1.0), writes=b_V2[i])
        EB2 = [sb(f"p3_EB{i}", [128, 2, 8, 2, 512], BF16) for i in range(2)]
        b_EB2 = [Buf(), Buf()]
        bstg = [sb(f"p3_bstg{i}", [128, 512], F32) for i in range(8)]
        b_bstg = [Buf() for _ in range(8)]
        pairs = list(pairs)
        bunits = [(v, t, hh) for v in range(2) for t in range(8) for hh in range(2)]

        def load_pair(pi_):
            j, pp = pairs[pi_], pi_ % 2
            P.dma("sync", KT2[pp][:], S["KTB"][j], writes=[b_KT2[pp]])
            P.dma("sync", QT2[pp][:], S["QTB"][j], writes=[b_QT2[pp]])
            vv = S["VB"][:, j * 128:(j + 1) * 128].rearrange("(kt p) (hh d) -> p kt hh d", p=128, hh=2)
            for hh in range(2):
                P.dma("sync", V2[pp][:, :, hh, 0:64], vv[:, :, hh, :], writes=[b_V2[pp][hh]])

        def bias_dma(pi_, g):
            j = pairs[pi_]
            for q in range(4):
                v, t, hh = bunits[g * 4 + q]
                bi = (g % 2) * 4 + q
                P.dma("sync", bstg[bi][:], I["nabias"][v, 2 * j + hh, t], writes=[b_bstg[bi]])

        def bias_exp(pi_, g):
            pp = pi_ % 2
            for q in range(4):
                v, t, hh = bunits[g * 4 + q]
                bi = (g % 2) * 4 + q
                P.op("act", lambda e, v=v, t=t, hh=hh, bi=bi: e.activation(out=EB2[pp][:, v, t, hh, :], in_=bstg[bi][:], func=AF.Exp),
                     reads=[b_bstg[bi]], writes=[b_EB2[pp]])
        pS = [ps(f"p3_pS{i}", [128, 1024], F32) for i in range(2)]
        b_pS = [Buf(), Buf()]
        pO = [ps(f"p3_pO{i}", [128, 512], F32) for i in range(2)]
        b_pO = Buf()
        pTr = ps("p3_pTr", [128, 8, 128], BF16)
        b_pTr = Buf()
        NPT = 3
        et = [sb(f"p3_et{i}", [128, 1024], BF16) for i in range(NPT)]
        b_et = [Buf() for _ in range(NPT)]
        pm = [sb(f"p3_pm{i}", [128, 1024], BF16) for i in range(NPT)]
        b_pm = [Buf() for _ in range(NPT)]
        rz = sb("p3_rz", [128, 8], F32)
        b_rz = Buf()
        onb = [sb(f"p3_on{i}", [128, 4, 128], BF16) for i in range(2)]
        b_on = [Buf(), Buf()]
        oT = [sb(f"p3_oT{i}", [128, 512], BF16) for i in range(2)]
        b_oT = [Buf(), Buf()]

        def acc(hh, qi):
            return pO[hh][:, qi * 65:qi * 65 + 65]

        fin = 0
        n = 0
        load_pair(0)
        bias_dma(0, 0)
        for g in range(8):
            if g + 1 < 8:
                bias_dma(0, g + 1)
            bias_exp(0, g)
        for pi_, j in enumerate(pairs):
            pp = pi_ % 2
            KT, QT, V, EB = KT2[pp], QT2[pp], V2[pp], EB2[pp]
            b_KT, b_QT, b_EB = b_KT2[pp], b_QT2[pp], b_EB2[pp]
            nxt = pi_ + 1 if pi_ + 1 < len(pairs) else None
            gd = ge = 0
            for si, s in enumerate(slots):
                if nxt is not None:
                    if si == 0:
                        load_pair(nxt)
                    if gd < 8:
                        bias_dma(nxt, gd)
                        gd += 1
                    if ge < gd - 1:
                        bias_exp(nxt, ge)
                        ge += 1
                v = 0 if s == 0 else 1
                KR0 = min(max(8 * s - 4, 0), 112)
                ktis = [KR0 // 2 + t for t in range(8)] + [36, 37]

                def emit_S(idx, sp, s=s, ktis=ktis):
                    kti = ktis[idx]
                    P.op("pe", lambda e, KT=KT, QT=QT: e.matmul(pS[sp][:, 0:512], lhsT=KT[0:64, kti * 128:(kti + 1) * 128],
                                                  rhs=QT[0:64, s * 512:(s + 1) * 512], start=True, stop=True, tile_position=(0, 0)),
                         reads=[b_KT, b_QT], writes=[b_pS[sp]], inc=False)
                    P.op("pe", lambda e, KT=KT, QT=QT: e.matmul(pS[sp][:, 512:1024], lhsT=KT[64:128, kti * 128:(kti + 1) * 128],
                                                  rhs=QT[64:128, s * 512:(s + 1) * 512], start=True, stop=True, tile_position=(64, 0)),
                         reads=[b_KT, b_QT], writes=[b_pS[sp]])

                emit_S(0, n % 2)
                for idx in range(10):
                    sp = n % 2
                    pi = n % NPT
                    n += 1
                    if idx + 1 < 10:
                        emit_S(idx + 1, n % 2)
                    kti = ktis[idx]
                    P.op("act", lambda e, sp=sp, pi=pi: e.activation(out=et[pi][:], in_=pS[sp][:], func=AF.Exp, scale=0.125),
                         reads=[b_pS[sp]], writes=[b_et[pi]])
                    if idx < 8:
                        P.op("dve", lambda e, pi=pi, v=v, idx=idx, EB=EB: e.tensor_tensor(out=pm[pi][:], in0=et[pi][:],
                                                                                   in1=EB[:, v, idx, :, :].rearrange("p a b -> p (a b)"), op=ALU.mult),
                             reads=[b_et[pi], b_EB], writes=[b_pm[pi]])
                        src_t, b_src = pm[pi], b_pm[pi]
                    else:
                        src_t, b_src = et[pi], b_et[pi]
                    for hh in range(2):
                        for qi in range(4):
                            P.op("pe", lambda e, hh=hh, qi=qi, src_t=src_t, kti=kti, idx=idx, V=V: e.matmul(
                                acc(hh, qi), lhsT=src_t[:, hh * 512 + qi * 128: hh * 512 + (qi + 1) * 128], rhs=V[:, kti, hh, 0:65],
                                start=(idx == 0 and qi == 0), stop=(idx == 9), skip_group_check=True),
                                 reads=[b_src] + b_V2[pp], writes=[b_pO], inc=(hh == 1 and qi == 3))
                oi = fin % 2
                fin += 1
                for hh in range(2):
                    for qi in range(4):
                        P.op("dve", lambda e, hh=hh, qi=qi: e.reciprocal(out=rz[:, hh * 4 + qi: hh * 4 + qi + 1], in_=acc(hh, qi)[:, 64:65]),
                             reads=[b_pO], writes=[b_rz])
                for hh in range(2):
                    for qi in range(4):
                        P.op("dve", lambda e, hh=hh, qi=qi, oi=oi: e.tensor_scalar(out=onb[oi][:, qi, hh * 64:(hh + 1) * 64], in0=acc(hh, qi)[:, 0:64],
                                                                                  scalar1=rz[:, hh * 4 + qi: hh * 4 + qi + 1], scalar2=None, op0=ALU.mult),
                             reads=[b_pO, b_rz], writes=[b_on[oi]])
                for qi in range(4):
                    P.op("pe", lambda e, qi=qi, oi=oi: e.transpose(pTr[:, qi, :], onb[oi][:, qi, :], K.identb[:]),
                         reads=[b_on[oi], K.b_identb], writes=[b_pTr], inc=(qi == 3))
                P.op("dve", lambda e, oi=oi: e.tensor_copy(oT[oi][:].rearrange("p (a b) -> p a b", b=128), pTr[:, 0:4, :]),
                     reads=[b_pTr], writes=[b_oT[oi]])
                P.dma("sync", S["OBT"][j][:, s * 512:(s + 1) * 512], oT[oi][:], reads=[b_oT[oi]])
            if nxt is not None:
                while ge < 8:
                    if gd < 8:
                        bias_dma(nxt, gd)
                        gd += 1
                    bias_exp(nxt, ge)
                    ge += 1
        P.wait_all_dma("sync")


BIG = 1.0e4


def phase4a(K, tiles=range(32), stop=99):
    nc, P, I, S = K.nc, K.P, K.I, K.S
    with contextlib.ExitStack() as st:
        sb = lambda n, s, d: st.enter_context(nc.sbuf_tensor(n, s, d))
        ps = lambda n, s, d: st.enter_context(nc.psum_tensor(n, s, d))
        Wa = sb("p4_Wa", [128, 8, D], BF16)
        Wbb = sb("p4_Wb", [128, 4, D], BF16)
        Wo = sb("p4_Wo", [128, 8, D], BF16)
        Wr = sb("p4_Wr", [128, 8, 36], F32)
        b_Wa = [[Buf(), Buf()] for _ in range(8)]
        b_Wb = [[Buf(), Buf()] for _ in range(4)]
        b_Wo = [[Buf(), Buf()] for _ in range(8)]
        b_Wr = Buf()
        wav = I["w_branch_a"].rearrange("(kc p) n -> p kc n", p=128)
        wbv = I["w_branch_b"].rearrange("(kc p) n -> p kc n", p=128)
        wov = I["w_out"].rearrange("(kc p) n -> p kc n", p=128)
        for kc in range(8):
            for c in range(2):
                P.dma("pool", Wa[:, kc, c * 512:(c + 1) * 512], wav[:, kc, c * 512:(c + 1) * 512], writes=[b_Wa[kc][c]])
        for kc in range(4):
            for c in range(2):
                P.dma("pool", Wbb[:, kc, c * 512:(c + 1) * 512], wbv[:, kc, c * 512:(c + 1) * 512], writes=[b_Wb[kc][c]])
        for kc in range(8):
            for c in range(2):
                P.dma("pool", Wo[:, kc, c * 512:(c + 1) * 512], wov[:, kc, c * 512:(c + 1) * 512], writes=[b_Wo[kc][c]])
        P.dma("sync", Wr[:], I["w_router"].rearrange("(kc p) n -> p kc n", p=128), writes=[b_Wr])
        ga1 = sb("p4_ga1", [128, D], F32)
        g2 = sb("p4_g2", [128, D], F32)
        sh2 = sb("p4_sh2", [128, D], F32)
        brt = sb("p4_brt", [128, 36], F32)
        b_ga1, b_g2, b_sh2, b_brt, b_c = Buf(), Buf(), Buf(), Buf(), Buf()
        xm = [sb(f"p4_xm{i}", [128, D], F32) for i in range(2)]
        b_xm = [Buf(), Buf()]
        load_bcast(K, P, ga1[:], S["M"][0:1, 2048:3072], b_ga1)
        load_bcast(K, P, sh2[:], S["M"][0:1, 3072:4096], b_sh2)
        load_bcast(K, P, g2[:], S["M"][0:1, 4096:5120], b_g2)
        load_bcast(K, P, xm[0][:], I["norm2_w"], b_xm[0])
        load_bcast(K, P, brt[:], I["b_router"], b_brt)
        P.op("dve", lambda e: e.scalar_tensor_tensor(out=g2[:], in0=g2[:], scalar=1.0, in1=xm[0][:], op0=ALU.add, op1=ALU.mult),
             reads=[b_g2, b_xm[0]], writes=[b_g2])
        nhalf = sb("p4_nhalf", [128, 1], F32)
        P.op("pool", lambda e: e.memset(nhalf[:], -0.5), writes=[b_c])

        NB = 4
        oaT = [sb(f"p4_oaT{i}", [128, 8, 128], BF16) for i in range(NB)]
        obT = [sb(f"p4_obT{i}", [128, 4, 128], BF16) for i in range(NB)]
        gat = [sb(f"p4_gat{i}", [128, D], BF16) for i in range(NB)]
        gbt = [sb(f"p4_gbt{i}", [128, D], BF16) for i in range(NB)]
        xt = [sb(f"p4_xt{i}", [128, D], F32) for i in range(NB)]
        b_oa, b_ob, b_ga, b_gb, b_xt = [[Buf() for _ in range(NB)] for _ in range(5)]
        t1 = sb("p4_t1", [128, D], F32)
        t2 = sb("p4_t2", [128, D], F32)
        t3 = sb("p4_t3", [128, D], F32)
        b_t1, b_t2, b_t3 = Buf(), Buf(), Buf()
        yb = sb("p4_yb", [128, D], BF16)
        b_yb = Buf()
        yT = sb("p4_yT", [128, 8, 128], BF16)
        b_yT = Buf()
        junk = sb("p4_junk", [128, D], BF16)
        b_junk = Buf()
        ss = sb("p4_ss", [128, 1], F32)
        rstd = sb("p4_rstd", [128, 1], F32)
        b_ss, b_rstd = Buf(), Buf()
        hx2 = sb("p4_hx2", [128, D], F32)
        b_hx2 = Buf()
        hTf = sb("p4_hTf", [128, 8, 128], F32)
        hxb = [sb(f"p4_hxb{i}", [128, D], BF16) for i in range(2)]
        b_hTf = Buf()
        b_hxb = [Buf(), Buf()]
        pA = ps("p4_pA", [128, D], F32)
        pB = ps("p4_pB", [128, D], F32)
        pC = ps("p4_pC", [128, D], F32)
        pY = ps("p4_pY", [128, 8, 128], BF16)
        pR = ps("p4_pR", [128, 512], F32)
        b_pA, b_pB, b_pC, b_pY, b_pR = Buf(), Buf(), Buf(), Buf(), Buf()
        lg = sb("p4_lg", [128, 36], F32)
        gmx = sb("p4_gmx", [128, 2], F32)
        gmask = sb("p4_gmask", [128, 4], F32)
        gj = sb("p4_gj", [128, 4], F32)
        gsum = sb("p4_gsum", [128, 1], F32)
        elm = sb("p4_elm", [128, 32], F32)
        top8 = sb("p4_top8", [128, 8], F32)
        wts = sb("p4_wts", [128, 4], F32)
        rt = [sb(f"p4_rt{i}", [128, 66], F32) for i in range(2)]
        b_r = Buf()
        b_rt = [Buf(), Buf()]
        tiles = list(tiles)

        def issue_loads(ti):
            if ti >= len(tiles):
                return
            t = tiles[ti]
            j = ti % NB
            u0 = t * 128
            P.dma("sync", oaT[j][:], S["OAT"][:, :, u0:u0 + 128].rearrange("h p t -> p h t"), writes=[b_oa[j]])
            P.dma("sync", obT[j][:], S["OBT"][:, :, u0:u0 + 128].rearrange("h p t -> p h t"), writes=[b_ob[j]])
            P.dma("sync", gat[j][:], S["GA"][u0:u0 + 128, :], writes=[b_ga[j]])
            P.dma("sync", gbt[j][:], S["GB"][u0:u0 + 128, :], writes=[b_gb[j]])
            P.dma("sync", xt[j][:], I["xall"][u0:u0 + 128, :], writes=[b_xt[j]])

        def stA(ti):
            j = ti % NB
            for c in range(2):
                for kc in range(8):
                    P.op("pe", lambda e, c=c, kc=kc: e.matmul(pA[:, c * 512:(c + 1) * 512], lhsT=oaT[j][:, kc, :], rhs=Wa[:, kc, c * 512:(c + 1) * 512],
                                                             start=(kc == 0), stop=(kc == 7)),
                         reads=[b_oa[j], b_Wa[kc][c]], writes=[b_pA], inc=(kc == 7 and c == 1))
            for c in range(2):
                for kc in range(4):
                    P.op("pe", lambda e, c=c, kc=kc: e.matmul(pB[:, c * 512:(c + 1) * 512], lhsT=obT[j][:, kc, :], rhs=Wbb[:, kc, c * 512:(c + 1) * 512],
                                                             start=(kc == 0), stop=(kc == 3)),
                         reads=[b_ob[j], b_Wb[kc][c]], writes=[b_pB], inc=(kc == 3 and c == 1))

        def stB(ti):
            j = ti % NB
            P.op("dve", lambda e: e.tensor_tensor(out=t1[:], in0=pA[:], in1=gat[j][:], op=ALU.mult), reads=[b_pA, b_ga[j]], writes=[b_t1])
            P.op("dve", lambda e: e.tensor_tensor(out=t2[:], in0=pB[:], in1=gbt[j][:], op=ALU.mult), reads=[b_pB, b_gb[j]], writes=[b_t2])
            P.op("pool", lambda e: e.tensor_tensor(out=yb[:], in0=t1[:], in1=t2[:], op=ALU.add), reads=[b_t1, b_t2], writes=[b_yb])

        def stC(ti):
            for kc in range(8):
                P.op("pe", lambda e, kc=kc: e.transpose(pY[:, kc, :], yb[:, kc * 128:(kc + 1) * 128], K.identb[:]),
                     reads=[b_yb, K.b_identb], writes=[b_pY], inc=(kc == 7))
            P.op("act", lambda e: e.copy(out=yT[:], in_=pY[:]), reads=[b_pY], writes=[b_yT])
            for c in range(2):
                for kc in range(8):
                    P.op("pe", lambda e, c=c, kc=kc: e.matmul(pC[:, c * 512:(c + 1) * 512], lhsT=yT[:, kc, :], rhs=Wo[:, kc, c * 512:(c + 1) * 512],
                                                             start=(kc == 0), stop=(kc == 7)),
                         reads=[b_yT, b_Wo[kc][c]], writes=[b_pC], inc=(kc == 7 and c == 1))

        def stD(ti):
            t = tiles[ti]
            j, d, u0 = ti % NB, ti % 2, t * 128
            P.op("dve", lambda e: e.tensor_tensor(out=t3[:], in0=pC[:], in1=ga1[:], op=ALU.mult), reads=[b_pC, b_ga1], writes=[b_t3])
            P.op("dve", lambda e: e.tensor_tensor(out=xm[d][:], in0=t3[:], in1=xt[j][:], op=ALU.add), reads=[b_t3, b_xt[j]], writes=[b_xm[d]])
            P.dma("sync", S["XMID"][u0:u0 + 128, :], xm[d][:], reads=[b_xm[d]])
            P.op("act", lambda e: e.activation(out=junk[:], in_=xm[d][:], func=AF.Square, accum_out=ss[:]),
                 reads=[b_xm[d]], writes=[b_junk, b_ss])
            P.op("dve", lambda e: e.tensor_scalar(out=ss[:], in0=ss[:], scalar1=1.0 / D, scalar2=EPS, op0=ALU.mult, op1=ALU.add),
                 reads=[b_ss], writes=[b_ss])
            P.op("pool", lambda e: e.tensor_tensor(out=rstd[:], in0=ss[:], in1=nhalf[:], op=ALU.pow), reads=[b_ss, b_c], writes=[b_rstd])
            P.op("dve", lambda e: e.scalar_tensor_tensor(out=hx2[:], in0=xm[d][:], scalar=rstd[:, 0:1], in1=g2[:], op0=ALU.mult, op1=ALU.mult),
                 reads=[b_xm[d], b_rstd, b_g2], writes=[b_hx2])
            P.op("dve", lambda e: e.tensor_tensor(out=hx2[:], in0=hx2[:], in1=sh2[:], op=ALU.add), reads=[b_hx2, b_sh2], writes=[b_hx2])

        def stE(ti):
            t = tiles[ti]
            d, u0 = ti % 2, t * 128
            pCv = pC[:].rearrange("p (a b) -> p a b", b=128)
            for kc in range(8):
                P.op("pe", lambda e, kc=kc: e.transpose(pCv[:, kc, :], hx2[:, kc * 128:(kc + 1) * 128], K.ident[:]),
                     reads=[b_hx2, K.b_ident], writes=[b_pC], inc=(kc == 7))
            P.op("act", lambda e: e.copy(out=hTf[:], in_=pCv), reads=[b_pC], writes=[b_hTf])
            P.op("act", lambda e: e.copy(out=hxb[d][:], in_=hx2[:]), reads=[b_hx2], writes=[b_hxb[d]])
            P.dma("sync", S["HX2"][u0:u0 + 128, :], hxb[d][:], reads=[b_hxb[d]])
            for kc in range(8):
                P.op("pe", lambda e, kc=kc: e.matmul(pR[:, 0:36], lhsT=hTf[:, kc, :], rhs=Wr[:, kc, :], start=(kc == 0), stop=(kc == 7)),
                     reads=[b_hTf, b_Wr], writes=[b_pR], inc=(kc == 7))

        def stF(ti):
            t = tiles[ti]
            d, u0 = ti % 2, t * 128
            R_ = [b_r]
            P.op("dve", lambda e: e.tensor_tensor(out=lg[:], in0=pR[:, 0:36], in1=brt[:], op=ALU.add), reads=[b_pR, b_brt], writes=R_)
            P.op("dve", lambda e: e.tensor_reduce(out=gmx[:, 0:1], in_=lg[:, 0:4], axis=AX.X, op=ALU.max), reads=R_, writes=R_)
            P.op("dve", lambda e: e.tensor_scalar(out=gmx[:, 1:2], in0=gmx[:, 0:1], scalar1=-1.0, scalar2=None, op0=ALU.mult), reads=R_, writes=R_)
            P.op("dve", lambda e: e.tensor_scalar(out=gmask[:], in0=lg[:, 0:4], scalar1=gmx[:, 0:1], scalar2=None, op0=ALU.is_equal), reads=R_, writes=R_)
            P.op("act", lambda e: e.activation(out=gj[:], in_=lg[:, 0:4], func=AF.Exp, bias=gmx[:, 1:2], scale=1.0, accum_out=gsum[:]), reads=R_, writes=R_)
            P.op("dve", lambda e: e.reciprocal(out=gsum[:], in_=gsum[:]), reads=R_, writes=R_)
            P.op("dve", lambda e: e.tensor_scalar(out=gmask[:], in0=gmask[:], scalar1=1.0, scalar2=BIG, op0=ALU.subtract, op1=ALU.mult), reads=R_, writes=R_)
            P.op("dve", lambda e: e.tensor_tensor(out=elm[:].rearrange("p (g x) -> p g x", x=8), in0=lg[:, 4:36].rearrange("p (g x) -> p g x", x=8),
                                                  in1=gmask[:].unsqueeze(2).to_broadcast([128, 4, 8]), op=ALU.add), reads=R_, writes=R_)
            P.op("dve", lambda e: e.max(out=top8[:], in_=elm[:]), reads=R_, writes=R_)
            Rd = [b_r, b_rt[d]]
            P.op("dve", lambda e: e.tensor_scalar(out=rt[d][:, 0:32], in0=elm[:], scalar1=top8[:, 0:1], scalar2=None, op0=ALU.is_equal), reads=R_, writes=Rd)
            P.op("dve", lambda e: e.tensor_scalar(out=rt[d][:, 32:64], in0=elm[:], scalar1=top8[:, 1:2], scalar2=None, op0=ALU.is_equal), reads=R_, writes=Rd)
            P.op("dve", lambda e: e.tensor_tensor(out=wts[:, 0:1], in0=top8[:, 1:2], in1=top8[:, 0:1], op=ALU.subtract), reads=R_, writes=R_)
            P.op("act", lambda e: e.activation(out=wts[:, 1:2], in_=wts[:, 0:1], func=AF.Exp), reads=R_, writes=R_)
            P.op("dve", lambda e: e.tensor_scalar(out=wts[:, 2:3], in0=wts[:, 1:2], scalar1=1.0, scalar2=None, op0=ALU.add), reads=R_, writes=R_)
            P.op("dve", lambda e: e.reciprocal(out=wts[:, 2:3], in_=wts[:, 2:3]), reads=R_, writes=R_)
            P.op("dve", lambda e: e.tensor_tensor(out=rt[d][:, 64:65], in0=wts[:, 2:3], in1=gsum[:], op=ALU.mult), reads=R_, writes=Rd)
            P.op("dve", lambda e: e.tensor_tensor(out=rt[d][:, 65:66], in0=rt[d][:, 64:65], in1=wts[:, 1:2], op=ALU.mult), reads=Rd, writes=Rd)
            P.dma("sync", S["ROUT"][u0:u0 + 128, :], rt[d][:], reads=[b_rt[d]])

        n = len(tiles)
        issue_loads(0)
        issue_loads(1)
        for ti in range(n + 1):
            issue_loads(ti + 2)
            if ti < n:
                stA(ti)
            if ti >= 1:
                stD(ti - 1)
                stE(ti - 1)
            if ti < n:
                stB(ti)
                stC(ti)
            if ti >= 1:
                stF(ti - 1)
        P.wait_all_dma("sync")


def phase4s(K, nslot=NSLOT, stop=99):
    nc, P, I, S = K.nc, K.P, K.I, K.S
    IOA = bass.IndirectOffsetOnAxis
    NW = 3
    with contextlib.ExitStack() as st:
        sb = lambda n, s, d: st.enter_context(nc.sbuf_tensor(n, s, d))
        ps = lambda n, s, d: st.enter_context(nc.psum_tensor(n, s, d))
        RT = sb("r_RT", [128, 32, 66], F32)
        b_RT = [Buf() for _ in range(4)]
        rv = S["ROUT"].rearrange("(j p) c -> p j c", p=128)
        for q in range(4):
            P.dma("sync", RT[:, q * 8:(q + 1) * 8, :], rv[:, q * 8:(q + 1) * 8, :], writes=[b_RT[q]])
        ga2 = sb("r_ga2", [128, D], F32)
        b_ga2 = Buf()
        load_bcast(K, P, ga2[:], S["M"][0:1, 5120:6144], b_ga2)
        b_c = Buf()
        onesb = sb("r_onesb", [128, 128], BF16)
        trif = sb("r_trif", [128, 128], F32)
        trib = sb("r_trib", [128, 128], BF16)
        thr32i = sb("r_thr32i", [128, 32], I32)
        thr96i = sb("r_thr96i", [128, NSLOT], I32)
        pidxi = sb("r_pidxi", [128, 1], I32)
        thr32 = sb("r_thr32", [128, 32], F32)
        thr96 = sb("r_thr96", [128, NSLOT], F32)
        pidx = sb("r_pidx", [128, 1], F32)
        tokid = sb("r_tokid", [128, 32], I32)
        zt = sb("r_zt", [NSLOT, 128], I32)
        P.op("pool", lambda e: e.memset(onesb[:], 1.0), writes=[b_c])
        trii = sb("r_trii", [128, 128], I32)
        P.op("pool", lambda e: e.iota(trii[:], pattern=[[1, 128]], base=0, channel_multiplier=-1), writes=[b_c])
        P.op("pool", lambda e: e.tensor_copy(trif[:], trii[:]), reads=[b_c], writes=[b_c])
        P.op("pool", lambda e: e.tensor_scalar(out=trib[:], in0=trif[:], scalar1=0.0, scalar2=None, op0=ALU.is_gt), reads=[b_c], writes=[b_c])
        P.op("pool", lambda e: e.iota(thr32i[:], pattern=[[128, 32]], base=0, channel_multiplier=0), writes=[b_c])
        P.op("pool", lambda e: e.iota(thr96i[:], pattern=[[128, NSLOT]], base=0, channel_multiplier=0), writes=[b_c])
        P.op("pool", lambda e: e.iota(pidxi[:], pattern=[[0, 1]], base=0, channel_multiplier=1), writes=[b_c])
        P.op("pool", lambda e: e.iota(tokid[:], pattern=[[128, 32]], base=0, channel_multiplier=1), writes=[b_c])
        P.op("pool", lambda e: e.memset(zt[:], 0), writes=[b_c])
        P.op("pool", lambda e: e.tensor_copy(thr32[:], thr32i[:]), reads=[b_c], writes=[b_c])
        P.op("pool", lambda e: e.tensor_copy(thr96[:], thr96i[:]), reads=[b_c], writes=[b_c])
        P.op("pool", lambda e: e.tensor_copy(pidx[:], pidxi[:]), reads=[b_c], writes=[b_c])
        b_tok0 = Buf()
        P.dma("sync", S["TOK"].rearrange("(a b) o -> a (b o)", b=128), zt[:], reads=[b_c], writes=[b_tok0])
        P.flush()

        maskb = sb("r_maskb", [128, 32, 32], BF16)
        b_mask = Buf()
        P.op("dve", lambda e: e.tensor_tensor(out=maskb[:], in0=RT[:, :, 0:32], in1=RT[:, :, 32:64], op=ALU.add),
             reads=b_RT, writes=[b_mask])
        pW = ps("r_pW", [128, 32, 32], F32)
        pTot = ps("r_pTot", [128, 32, 32], F32)
        b_pW, b_pTot = Buf(), Buf()
        mflat = maskb[:].rearrange("p j e -> p (j e)")
        ptflat = pTot[:].rearrange("p j e -> p (j e)")
        for c in range(2):
            P.op("pe", lambda e, c=c: e.matmul(ptflat[:, c * 512:(c + 1) * 512], lhsT=onesb[:], rhs=mflat[:, c * 512:(c + 1) * 512],
                                               start=True, stop=True), reads=[b_c, b_mask], writes=[b_pTot], inc=(c == 1))
        for j in range(32):
            P.op("pe", lambda e, j=j: e.matmul(pW[:, j, :], lhsT=trib[:], rhs=maskb[:, j, :], start=True, stop=True),
                 reads=[b_c, b_mask], writes=[b_pW], inc=(j == 31))
        cs = [sb(f"r_cs{i}", [128, 32, 32], F32) for i in range(2)]
        b_cs = [Buf(), Buf()]
        P.op("act", lambda e: e.copy(out=cs[0][:], in_=pTot[:]), reads=[b_pTot], writes=[b_cs[0]])
        a = 0
        for sft in (1, 2, 4, 8, 16):
            P.op("dve", lambda e, a=a, sft=sft: e.tensor_tensor(out=cs[1 - a][:, sft:, :], in0=cs[a][:, sft:, :], in1=cs[a][:, :32 - sft, :], op=ALU.add),
                 reads=[b_cs[a]], writes=[b_cs[1 - a]])
            P.op("dve", lambda e, a=a, sft=sft: e.tensor_copy(cs[1 - a][:, :sft, :], cs[a][:, :sft, :]), reads=[b_cs[a]], writes=[b_cs[1 - a]])
            a = 1 - a
        incl, b_incl = cs[a], b_cs[a]
        R, b_R = cs[1 - a], b_cs[1 - a]
        G = sb("r_G", [128, 32, 32], F32)
        b_G = Buf()
        sm = Buf()
        ce = sb("r_ce", [128, 32], F32)
        pad = [sb(f"r_pad{i}", [128, 32], F32) for i in range(2)]
        base = sb("r_base", [128, 32], F32)
        P.op("dve", lambda e: e.tensor_tensor(out=R[:], in0=incl[:], in1=pTot[:], op=ALU.subtract), reads=[b_incl, b_pTot], writes=[b_R])
        P.op("dve", lambda e: e.tensor_tensor(out=R[:], in0=R[:], in1=pW[:], op=ALU.add), reads=[b_R, b_pW], writes=[b_R])
        P.op("dve", lambda e: e.tensor_tensor(out=G[:], in0=incl[:, 31, :].unsqueeze(2).to_broadcast([128, 32, 32]),
                                              in1=thr32[:].unsqueeze(1).to_broadcast([128, 32, 32]), op=ALU.is_gt),
             reads=[b_incl, b_c], writes=[b_G])
        P.op("dve", lambda e: e.tensor_reduce(out=ce[:], in_=G[:], axis=AX.X, op=ALU.add), reads=[b_G], writes=[sm])
        P.op("dve", lambda e: e.tensor_scalar(out=pad[0][:], in0=ce[:], scalar1=128.0, scalar2=None, op0=ALU.mult), reads=[sm], writes=[sm])
        P.op("dve", lambda e: e.tensor_copy(base[:], pad[0][:]), reads=[sm], writes=[sm])
        a2 = 0
        for sft in (1, 2, 4, 8, 16):
            P.op("dve", lambda e, a2=a2, sft=sft: e.tensor_tensor(out=pad[1 - a2][:, sft:], in0=pad[a2][:, sft:], in1=pad[a2][:, :32 - sft], op=ALU.add),
                 reads=[sm], writes=[sm])
            P.op("dve", lambda e, a2=a2, sft=sft: e.tensor_copy(pad[1 - a2][:, :sft], pad[a2][:, :sft]), reads=[sm], writes=[sm])
            a2 = 1 - a2
        endv = pad[a2]
        P.op("dve", lambda e: e.tensor_tensor(out=base[:], in0=endv[:], in1=base[:], op=ALU.subtract), reads=[sm], writes=[sm])
        P.op("dve", lambda e: e.tensor_tensor(out=R[:], in0=R[:], in1=base[:].unsqueeze(1).to_broadcast([128, 32, 32]), op=ALU.add),
             reads=[b_R, sm], writes=[b_R])
        posf = sb("r_posf", [128, 2, 32], F32)
        posi = sb("r_posi", [128, 64], I32)
        b_pos = Buf()
        for k in range(2):
            P.op("dve", lambda e, k=k: e.tensor_tensor(out=G[:], in0=R[:], in1=RT[:, :, k * 32:(k + 1) * 32], op=ALU.mult),
                 reads=[b_R] + b_RT, writes=[b_G])
            P.op("dve", lambda e, k=k: e.tensor_reduce(out=posf[:, k, :], in_=G[:], axis=AX.X, op=ALU.add), reads=[b_G], writes=[b_pos])
        P.op("dve", lambda e: e.tensor_copy(posi[:], posf[:].rearrange("p k j -> p (k j)")), reads=[b_pos], writes=[b_pos])
        G2 = sb("r_G2", [128, NSLOT, 32], F32)
        eid = sb("r_eid", [128, NSLOT], F32)
        idxf = sb("r_idxf", [128, NSLOT], F32)
        idxw = sb("r_idxw", [128, NSLOT], I32)
        b_idx = Buf()
        P.op("dve", lambda e: e.tensor_tensor(out=G2[:], in0=endv[:].unsqueeze(1).to_broadcast([128, NSLOT, 32]),
                                              in1=thr96[:].unsqueeze(2).to_broadcast([128, NSLOT, 32]), op=ALU.is_le),
             reads=[sm, b_c], writes=[b_idx])
        P.op("dve", lambda e: e.tensor_reduce(out=eid[:], in_=G2[:], axis=AX.X, op=ALU.add), reads=[b_idx], writes=[b_idx])
        P.op("dve", lambda e: e.scalar_tensor_tensor(out=idxf[:], in0=eid[:], scalar=128.0, in1=pidx[:].to_broadcast([128, NSLOT]),
                                                     op0=ALU.mult, op1=ALU.add), reads=[b_idx, b_c], writes=[b_idx])
        P.op("dve", lambda e: e.tensor_copy(idxw[:], idxf[:]), reads=[b_idx], writes=[b_idx])
        if "DBGI" in S:
            P.dma("sync", S["DBGI"][:, 0:64], posi[:], reads=[b_pos])
            P.dma("sync", S["DBGI"][:, 64:64 + NSLOT], idxw[:], reads=[b_idx])
            P.dma("sync", S["DBGF"][:, 0:32], incl[:, 31, :], reads=[b_incl])
            P.dma("sync", S["DBGF"][:, 32:64], endv[:], reads=[sm])
            P.dma("sync", S["DBGF"][:, 64:64 + NSLOT], eid[:], reads=[b_idx])
        if stop <= 1:
            P.wait_all_dma("sync")
            return
        b_sc = [Buf() for _ in range(64)]
        for k in range(2):
            for j in range(32):
                P.idma(lambda e, k=k, j=j: e.indirect_dma_start(out=S["TOK"], out_offset=IOA(ap=posi[:, k * 32 + j:k * 32 + j + 1], axis=0),
                                                                in_=tokid[:, j:j + 1], in_offset=None, bounds_check=None, oob_is_err=False),
                       reads=[b_pos, b_c, b_tok0], writes=[b_sc[k * 32 + j]])
        P.flush()
        tokall = sb("r_tokall", [128, NSLOT], I32)
        b_tok = [Buf() for _ in range(NSLOT)]
        for i in range(nslot):
            P.dma("sync", tokall[:, i:i + 1], S["TOK"][i * 128:(i + 1) * 128, :], writes=[b_tok[i]])

        if "DBGI" in S:
            P.dma("sync", S["DBGI"][:, 64 + 2 * NSLOT:64 + 3 * NSLOT], tokall[:], reads=b_tok)
        if stop <= 2:
            P.wait_all_dma("sync")
            return
        Wg = [sb(f"m_Wg{i}", [128, 8, 512], BF16) for i in range(NW)]
        Wu = [sb(f"m_Wu{i}", [128, 8, 512], BF16) for i in range(NW)]
        Wd = [sb(f"m_Wd{i}", [128, 4, D], BF16) for i in range(NW)]
        b_Wg = [Buf() for _ in range(NW)]
        b_Wu = [Buf() for _ in range(NW)]
        b_Wd = [Buf() for _ in range(NW)]
        hxg = [sb(f"m_hxg{i}", [128, D], BF16) for i in range(NW)]
        b_hxg = [Buf() for _ in range(NW)]
        hT = [sb(f"m_hT{i}", [128, 8, 128], BF16) for i in range(2)]
        b_hT = [Buf(), Buf()]
        sg = [sb(f"m_sg{i}", [128, 512], BF16) for i in range(2)]
        b_sg = [Buf(), Buf()]
        at = [sb(f"m_at{i}", [128, 512], BF16) for i in range(2)]
        b_at = [Buf(), Buf()]
        aT = [sb(f"m_aT{i}", [128, 4, 128], BF16) for i in range(2)]
        b_aT = [Buf(), Buf()]
        ys = [sb(f"m_ys{i}", [128, D], F32) for i in range(2)]
        b_ys = [Buf(), Buf()]
        pTh = ps("m_pTh", [128, 8, 128], BF16)
        pTa = ps("m_pTa", [128, 8, 128], BF16)
        pg = [ptflat[:, i * 512:(i + 1) * 512] for i in range(2)]
        pu = [ps(f"m_pu{i}", [128, 512], F32) for i in range(2)]
        b_pTh, b_pTa = Buf(), Buf()
        b_pg, b_pu = [b_pTot, Buf()], [Buf(), Buf()]
        py = pW
        pyf = pW[:].rearrange("p j e -> p (j e)")
        b_py = b_pW
        regw = nc.alloc_register(mybir.EngineType.Pool, "moe_bc_w")
        bc = {}

        def set_bounds(e):
            e.reg_mov(regw, 32 * 128 - 1)
            bc['w'] = e.snap(regw)
        P.q["pool"].append(set_bounds)

        def issue_gathers(i):
            w = i % NW
            P.idma(lambda e: e.indirect_dma_start(out=hxg[w][:], out_offset=None, in_=S["HX2"], in_offset=IOA(ap=tokall[:, i:i + 1], axis=0),
                                                  bounds_check=None, oob_is_err=False), reads=[b_tok[i]], writes=[b_hxg[w]])
            for Wt, b_Wt, name in ((Wg, b_Wg, "WG16"), (Wu, b_Wu, "WU16"), (Wd, b_Wd, "WD16")):
                P.idma(lambda e, Wt=Wt, name=name: e.indirect_dma_start(out=Wt[w][:].rearrange("p a b -> p (a b)"), out_offset=None, in_=S[name],
                                                                        in_offset=IOA(ap=idxw[:, i:i + 1], axis=0),
                                                                        bounds_check=bc['w'], oob_is_err=False),
                       reads=[b_idx], writes=[b_Wt[w]])

        for i in range(min(NW - 1, nslot)):
            issue_gathers(i)
        for i in range(nslot):
            w, d = i % NW, i % 2
            if i + NW - 1 < nslot:
                issue_gathers(i + NW - 1)
            for kc in range(8):
                P.op("pe", lambda e, kc=kc, w=w: e.transpose(pTh[:, kc, :], hxg[w][:, kc * 128:(kc + 1) * 128], K.identb[:]),
                     reads=[b_hxg[w], K.b_identb], writes=[b_pTh], inc=(kc == 7))
            P.op("act", lambda e, d=d: e.copy(out=hT[d][:], in_=pTh[:]), reads=[b_pTh], writes=[b_hT[d]])
            for kc in range(8):
                P.op("pe", lambda e, kc=kc, w=w, d=d: e.matmul(pg[d], lhsT=hT[d][:, kc, :], rhs=Wg[w][:, kc, :], start=(kc == 0), stop=(kc == 7)),
                     reads=[b_hT[d], b_Wg[w]], writes=[b_pg[d]], inc=(kc == 7))
            for kc in range(8):
                P.op("pe", lambda e, kc=kc, w=w, d=d: e.matmul(pu[d][:], lhsT=hT[d][:, kc, :], rhs=Wu[w][:, kc, :], start=(kc == 0), stop=(kc == 7)),
                     reads=[b_hT[d], b_Wu[w]], writes=[b_pu[d]], inc=(kc == 7))
            P.op("act", lambda e, d=d: e.activation(out=sg[d][:], in_=pg[d], func=AF.Silu), reads=[b_pg[d]], writes=[b_sg[d]])
            P.op("dve", lambda e, d=d: e.tensor_tensor(out=at[d][:], in0=sg[d][:], in1=pu[d][:], op=ALU.mult), reads=[b_sg[d], b_pu[d]], writes=[b_at[d]])
            for hc in range(4):
                P.op("pe", lambda e, hc=hc, d=d: e.transpose(pTa[:, hc, :], at[d][:, hc * 128:(hc + 1) * 128], K.identb[:]),
                     reads=[b_at[d], K.b_identb], writes=[b_pTa], inc=(hc == 3))
            P.op("act", lambda e, d=d: e.copy(out=aT[d][:], in_=pTa[:, 0:4, :]), reads=[b_pTa], writes=[b_aT[d]])
            for c in range(2):
                for hc in range(4):
                    P.op("pe", lambda e, c=c, hc=hc, w=w, d=d: e.matmul(pyf[:, c * 512:(c + 1) * 512], lhsT=aT[d][:, hc, :], rhs=Wd[w][:, hc, c * 512:(c + 1) * 512],
                                                                       start=(hc == 0), stop=(hc == 3)),
                         reads=[b_aT[d], b_Wd[w]], writes=[b_py], inc=(hc == 3 and c == 1))
            P.op("dve", lambda e, d=d: e.tensor_copy(ys[d][:], pyf), reads=[b_py], writes=[b_ys[d]])
            P.dma("sync", S["Y"][i * 128:(i + 1) * 128, :], ys[d][:], reads=[b_ys[d]])
        P.flush()
        if stop <= 3:
            return

        xm = [sb(f"c_xm{i}", [128, D], F32) for i in range(2)]
        y0 = [sb(f"c_y0{i}", [128, D], F32) for i in range(2)]
        y1 = [sb(f"c_y1{i}", [128, D], F32) for i in range(2)]
        tt = [sb(f"c_t{i}", [128, D], F32) for i in range(2)]
        ot = [sb(f"c_o{i}", [128, D], F32) for i in range(2)]
        b_xm, b_y0, b_y1, b_tt, b_ot = [[Buf(), Buf()] for _ in range(5)]

        def issue_c(j):
            d = j % 2
            P.dma("sync", xm[d][:], S["XMID"][j * 128:(j + 1) * 128, :], writes=[b_xm[d]])
            P.idma(lambda e: e.indirect_dma_start(out=y0[d][:], out_offset=None, in_=S["Y"], in_offset=IOA(ap=posi[:, j:j + 1], axis=0),
                                                  bounds_check=None, oob_is_err=False), reads=[b_pos], writes=[b_y0[d]])
            P.idma(lambda e: e.indirect_dma_start(out=y1[d][:], out_offset=None, in_=S["Y"], in_offset=IOA(ap=posi[:, 32 + j:33 + j], axis=0),
                                                  bounds_check=None, oob_is_err=False), reads=[b_pos], writes=[b_y1[d]])

        issue_c(0)
        for j in range(32):
            d = j % 2
            if j + 1 < 32:
                issue_c(j + 1)
            P.op("dve", lambda e, j=j, d=d: e.tensor_scalar(out=tt[d][:], in0=y0[d][:], scalar1=RT[:, j, 64:65], scalar2=None, op0=ALU.mult),
                 reads=[b_y0[d]] + b_RT, writes=[b_tt[d]])
            P.op("dve", lambda e, j=j, d=d: e.scalar_tensor_tensor(out=tt[d][:], in0=y1[d][:], scalar=RT[:, j, 65:66], in1=tt[d][:], op0=ALU.mult, op1=ALU.add),
                 reads=[b_y1[d], b_tt[d]] + b_RT, writes=[b_tt[d]])
            P.op("dve", lambda e, d=d: e.tensor_tensor(out=tt[d][:], in0=tt[d][:], in1=ga2[:], op=ALU.mult), reads=[b_tt[d], b_ga2], writes=[b_tt[d]])
            P.op("dve", lambda e, d=d: e.tensor_tensor(out=ot[d][:], in0=tt[d][:], in1=xm[d][:], op=ALU.add), reads=[b_tt[d], b_xm[d]], writes=[b_ot[d]])
            P.dma("sync", K.out[j * 128:(j + 1) * 128, :], ot[d][:], reads=[b_ot[d]])
        P.wait_all_dma("sync")


def _rope_tables(half):
    u = np.arange(NT)
    l = u // 64
    w = u % 64
    r = l if half == 0 else 127 - l
    pos = np.stack([r, w], axis=-1).astype(np.float32)
    inv_freq = (np.float32(10000.0) ** (-np.arange(16, dtype=np.float32) / np.float32(16))).astype(np.float32)
    ang = (pos[:, :, None] * inv_freq).astype(np.float32)
    cos = np.cos(ang).astype(np.float32)
    sin = np.sin(ang).astype(np.float32)
    C = np.ones((NKA, 2, 2, 16), np.float32)
    Sg = np.zeros((NKA, 2, 2, 16), np.float32)
    C[:NT, :, 0, :] = cos
    C[:NT, :, 1, :] = cos
    Sg[:NT, :, 0, :] = -sin
    Sg[:NT, :, 1, :] = sin
    return C.reshape(NKA, 64), Sg.reshape(NKA, 64)


def _na_bias_tables(rel_bias, half):
    pad = np.concatenate([rel_bias.reshape(8, -1), np.full((8, 1), NEG, np.float32)], axis=1)
    out = np.empty((2, 8, 8, 128, 512), np.float32)
    for v, slot in enumerate((0, 3)):
        R = 8 * slot
        KR0 = min(max(R - 4, 0), 112)
        ql = R + np.arange(8)
        kl = KR0 + np.arange(16)
        g = (lambda a: a) if half == 0 else (lambda a: 127 - a)
        qg = g(ql)[None, None, :, None]
        kg = g(kl)[:, None, None, None]
        kc = np.arange(64)[None, :, None, None]
        qw = np.arange(64)[None, None, None, :]
        r0 = np.clip(qg - 4, 0, 120)
        c0 = np.clip(qw - 8, 0, 48)
        valid = (kg >= r0) & (kg <= r0 + 7) & (kc >= c0) & (kc <= c0 + 15)
        idx = (kg - qg + 7) * 31 + (kc - qw + 15)
        idx = np.where(valid, idx, 465)
        idx = np.broadcast_to(idx, (16, 64, 8, 64)).reshape(8, 128, 512)
        out[v] = pad[:, idx]
    return out


def prep_core_inputs(core, inp):
    b, half = core // 2, core % 2
    xb = np.asarray(inp["x"][b])
    if half == 1:
        xb = xb.reshape(128, 64, D)[::-1].reshape(NT, D)
    xall = np.ascontiguousarray(np.concatenate([xb, np.asarray(inp["ctx"][b])], axis=0))
    C, Sg = _rope_tables(half)
    m = {
        "xall": xall,
        "cc": np.ascontiguousarray(np.stack([np.asarray(inp["c"][b]), np.asarray(inp["c_ctx"])], axis=0)),
        "ropec": C, "ropes": Sg,
        "w_ada": np.asarray(inp["w_ada"][0]), "b_ada": np.asarray(inp["b_ada"][0]).reshape(1, -1),
        "norm1_w": np.asarray(inp["norm1_w"][0]).reshape(1, -1), "norm2_w": np.asarray(inp["norm2_w"][0]).reshape(1, -1),
        "w_in": np.asarray(inp["w_in"][0]),
        "subln_a": np.asarray(inp["subln_a"][0]).reshape(1, -1),
        "nabias": _na_bias_tables(np.asarray(inp["na_rel_bias"][0]), half),
        "w_branch_a": np.asarray(inp["w_branch_a"][0]), "w_branch_b": np.asarray(inp["w_branch_b"][0]),
        "w_out": np.asarray(inp["w_out"][0]),
        "w_router": np.ascontiguousarray(np.concatenate(
            [np.asarray(inp["w_router_group"][0])] + [np.asarray(inp["w_router_expert"][0][g]) for g in range(4)], axis=1)),
        "b_router": np.concatenate([np.asarray(inp["b_router_group"][0]).reshape(-1),
                                    np.asarray(inp["b_router_expert"][0]).reshape(-1)]).reshape(1, 36),
        "w_expert_gate": np.asarray(inp["w_expert_gate"][0]), "w_expert_up": np.asarray(inp["w_expert_up"][0]),
        "w_expert_down": np.asarray(inp["w_expert_down"][0]),
    }
    for n in ("q_norm_a", "k_norm_a", "lambda_q1", "lambda_k1", "lambda_q2", "lambda_k2", "q_norm_b", "k_norm_b"):
        m[n] = np.asarray(inp[n][0]).reshape(1, -1)
    return {k: np.ascontiguousarray(v, dtype=np.float32) for k, v in m.items()}


def kernel(**inputs):
    nc, _ = build()
    in_maps = [prep_core_inputs(c, inputs) for c in range(8)]
    res = run_bass_kernel_spmd(nc, in_maps, core_ids=list(range(8)))
    outp = np.empty((4, NT, D), np.float32)
    for c in range(8):
        b, half = c // 2, c % 2
        o = np.asarray(res.results[c]["out"]).reshape(64, 64, D)
        if half == 0:
            outp[b, :NOWN] = o.reshape(NOWN, D)
        else:
            outp[b, NOWN:] = o[::-1].reshape(NOWN, D)
    return outp
```

```python
import contextlib
import numpy as np
import concourse.bass as bass
import concourse.mybir as mybir
from concourse.bass_utils import run_bass_kernel_spmd

F32 = mybir.dt.float32
BF16 = mybir.dt.bfloat16
I32 = mybir.dt.int32
AF = mybir.ActivationFunctionType
ALU = mybir.AluOpType
AX = mybir.AxisListType

D = 1024
NT = 8192
NOWN = 4096
NCTX = 256
NKA = NT + NCTX
NKB = 4608 + NCTX
INW = 6656
EPS = 1e-6
NEG = -30000.0
SEM_LIMIT = 8000
NSLOT = 96


class Buf:
    __slots__ = ("name", "lw", "rd")

    def __init__(self, name=""):
        self.name = name
        self.lw = None
        self.rd = []


class Prog:
    ENGS = ("sync", "act", "pool", "pe", "dve")

    def __init__(self, nc, stack):
        self.nc = nc
        self.stack = stack
        self.q = {e: [] for e in self.ENGS}
        self.cur = {}
        self.cnt = {}
        self.nsem = 0
        for e in ("act", "pool", "pe", "dve"):
            self._newsem(e)
        self.waited = {e: {} for e in self.ENGS}
        self.dma_pool = []
        self.dma_i = 0
        self.ninstr = 0

    def _mksem(self, name):
        self.nsem += 1
        return self.stack.enter_context(self.nc.semaphore(f"{name}_{self.nsem}"))

    def _newsem(self, e):
        self.cur[e] = self._mksem("s" + e)
        self.cnt[e] = 0

    def init_dma_pool(self, n, n_sw=8):
        for _ in range(n):
            self.dma_pool.append([self._mksem("dma"), 0])
        self.sw_pool = [[self._mksem("swdma"), 0] for _ in range(n_sw)]
        self.sw_i = 0

    def _dma_slot(self, eng):
        if eng == "pool":
            slot = self.sw_pool[self.sw_i % len(self.sw_pool)]
            self.sw_i += 1
        else:
            slot = self.dma_pool[self.dma_i % len(self.dma_pool)]
            self.dma_i += 1
        return slot

    def _wait(self, eng, h):
        if h is None:
            return
        sem, val, _ = h
        w = self.waited[eng]
        k = id(sem)
        if w.get(k, (None, 0))[1] >= val:
            return
        w[k] = (sem, val)
        self.q[eng].append(lambda e, sem=sem, val=val: e.wait_ge(sem, val))
        self.ninstr += 1

    def _deps(self, eng, reads, writes, extra):
        for b in reads:
            self._wait(eng, b.lw)
        for b in writes:
            if b.lw is not None and (b.lw[2] != eng or eng != "pe"):
                self._wait(eng, b.lw)
            for h in b.rd:
                if h[2] != eng or eng != "pe":
                    self._wait(eng, h)
        for h in extra:
            self._wait(eng, h)

    def _commit(self, h, reads, writes):
        for b in writes:
            b.lw = h
            b.rd = []
        for b in reads:
            b.rd.append(h)
            if len(b.rd) > 32:
                b.rd = b.rd[-32:]

    def op(self, eng, fn, reads=(), writes=(), extra=(), inc=True):
        self._deps(eng, reads, writes, extra)
        self.ninstr += 1
        if not inc:
            self.q[eng].append(lambda e, fn=fn: fn(e))
            return None
        if self.cnt[eng] >= SEM_LIMIT:
            self._newsem(eng)
        self.cnt[eng] += 1
        sem = self.cur[eng]
        h = (sem, self.cnt[eng], eng)
        self.q[eng].append(lambda e, fn=fn, sem=sem: fn(e).then_inc(sem, 1))
        self._commit(h, reads, writes)
        return h

    def dma(self, eng, out, in_, reads=(), writes=(), extra=(), **kw):
        self._deps(eng, reads, writes, extra)
        slot = self._dma_slot(eng)
        if slot[1] > 0:
            self._wait(eng, (slot[0], slot[1], "dma"))
        if slot[1] >= SEM_LIMIT * 16:
            slot[0] = self._mksem("dma")
            slot[1] = 0
        sem = slot[0]
        slot[1] += 16
        h = (sem, slot[1], "dma")
        self.q[eng].append(lambda e, out=out, in_=in_, sem=sem, kw=kw:
                           e.dma_start(out=out, in_=in_, **kw).then_inc(sem, 16))
        self.ninstr += 1
        self._commit(h, reads, writes)
        return h

    def idma(self, fn, reads=(), writes=(), eng="pool"):
        self._deps(eng, reads, writes, ())
        slot = self._dma_slot(eng)
        if slot[1] > 0:
            self._wait(eng, (slot[0], slot[1], "dma"))
        if slot[1] >= SEM_LIMIT * 16:
            slot[0] = self._mksem("dma")
            slot[1] = 0
        sem = slot[0]
        slot[1] += 16
        h = (sem, slot[1], "dma")
        self.q[eng].append(lambda e, fn=fn, sem=sem: fn(e).then_inc(sem, 16))
        self.ninstr += 1
        self._commit(h, reads, writes)
        return h

    def wait_all_dma(self, eng="sync"):
        for slot in self.dma_pool + self.sw_pool:
            if slot[1] > 0:
                self._wait(eng, (slot[0], slot[1], "dma"))

    def flush(self):
        self.wait_all_dma("sync")
        with self.nc.Block() as block:
            m = {"sync": block.sync, "act": block.scalar, "pool": block.gpsimd,
                 "pe": block.tensor, "dve": block.vector}
            for e in self.ENGS:
                fns = self.q[e]
                if not fns:
                    continue

                def body(engine, fns=fns):
                    for f in fns:
                        f(engine)
                m[e](body)
        self.q = {e: [] for e in self.ENGS}


class Ctx:
    pass


def build(debug=(), upto=99, opts=None):
    opts = opts or {}
    nc = bass.Bass("TRN2", target_bir_lowering=False)
    K = Ctx()
    K.nc = nc

    def din(name, shape, dt=F32):
        return nc.dram_tensor(name, list(shape), dt, kind="ExternalInput").ap()

    def dscr(name, shape, dt):
        kind = "ExternalOutput" if name in debug else "Internal"
        return nc.dram_tensor(name, list(shape), dt, kind=kind).ap()

    I = {}
    I["xall"] = din("xall", [NKA, D])
    I["cc"] = din("cc", [2, D])
    I["ropec"] = din("ropec", [NKA, 64])
    I["ropes"] = din("ropes", [NKA, 64])
    I["w_ada"] = din("w_ada", [D, 6 * D])
    I["b_ada"] = din("b_ada", [1, 6 * D])
    I["norm1_w"] = din("norm1_w", [1, D])
    I["norm2_w"] = din("norm2_w", [1, D])
    I["w_in"] = din("w_in", [D, INW])
    for n in ("q_norm_a", "k_norm_a", "lambda_q1", "lambda_k1", "lambda_q2", "lambda_k2", "q_norm_b", "k_norm_b"):
        I[n] = din(n, [1, 64])
    I["subln_a"] = din("subln_a", [1, 128])
    I["nabias"] = din("nabias", [2, 8, 8, 128, 512])
    I["w_branch_a"] = din("w_branch_a", [D, D])
    I["w_branch_b"] = din("w_branch_b", [512, D])
    I["w_out"] = din("w_out", [D, D])
    I["w_router"] = din("w_router", [D, 36])
    I["b_router"] = din("b_router", [1, 36])
    I["w_expert_gate"] = din("w_expert_gate", [32, D, 512])
    I["w_expert_up"] = din("w_expert_up", [32, D, 512])
    I["w_expert_down"] = din("w_expert_down", [32, 512, D])
    out = nc.dram_tensor("out", [NOWN, D], F32, kind="ExternalOutput").ap()

    S = {}
    S["M"] = dscr("M", [2, 6 * D], F32)
    S["QTA"] = dscr("QTA", [8, 128, NOWN], BF16)
    S["KTA"] = dscr("KTA", [8, 128, NKA], BF16)
    S["VA"] = dscr("VA", [NKA, 1024], BF16)
    S["QTB"] = dscr("QTB", [4, 128, NOWN], BF16)
    S["KTB"] = dscr("KTB", [4, 128, NKB], BF16)
    S["VB"] = dscr("VB", [NKB, 512], BF16)
    S["GA"] = dscr("GA", [NOWN, 1024], BF16)
    S["GB"] = dscr("GB", [NOWN, 1024], BF16)
    S["OAT"] = dscr("OAT", [8, 128, NOWN], BF16)
    S["OBT"] = dscr("OBT", [4, 128, NOWN], BF16)
    S["XMID"] = dscr("XMID", [NOWN, D], F32)
    S["HX2"] = dscr("HX2", [NOWN, D], BF16)
    S["ROUT"] = dscr("ROUT", [NOWN, 66], F32)
    S["TOK"] = dscr("TOK", [NSLOT * 128, 1], I32)
    S["Y"] = dscr("Y", [NSLOT * 128, D], F32)
    S["WG16"] = dscr("WG16", [32 * 128, 8 * 512], BF16)
    S["WU16"] = dscr("WU16", [32 * 128, 8 * 512], BF16)
    S["WD16"] = dscr("WD16", [32 * 128, 4 * D], BF16)
    if "DBGI" in debug:
        S["DBGI"] = dscr("DBGI", [128, 64 + 3 * NSLOT], I32)
        S["DBGF"] = dscr("DBGF", [128, 64 + NSLOT], F32)
    K.I, K.S, K.out = I, S, out

    with contextlib.ExitStack() as gst:
        P = Prog(nc, gst)
        P.init_dma_pool(12)
        K.P = P
        K.ident = gst.enter_context(nc.sbuf_tensor("ident", [128, 128], F32))
        K.identb = gst.enter_context(nc.sbuf_tensor("identb", [128, 128], BF16))
        K.b_ident = Buf("ident")
        K.b_identb = Buf("identb")
        P.op("pool", lambda e: e.memset(K.ident[:], 0.0), writes=[K.b_ident])
        P.op("pool", lambda e: e.affine_select(out=K.ident[:], in_=K.ident[:], pattern=[[-1, 128]],
                                               compare_op=ALU.not_equal, fill=1.0, base=0, channel_multiplier=1),
             reads=[K.b_ident], writes=[K.b_ident])
        P.op("pool", lambda e: e.tensor_copy(K.identb[:], K.ident[:]), reads=[K.b_ident], writes=[K.b_identb])

        phase0(K)
        P.flush()
        if upto >= 1:
            phase1(K)
            P.flush()
        if upto >= 2:
            phase2(K, **opts.get('p2', {}))
            P.flush()
        if upto >= 3:
            phase3(K, **opts.get('p3', {}))
            P.flush()
        if upto >= 4:
            phase4a(K, **opts.get('p4a', {}))
            P.flush()
        if upto >= 5:
            phase4s(K, **opts.get('p4s', {}))
            P.flush()
        K.ninstr = P.ninstr
    return nc, K


def phase0(K):
    nc, P, I, S = K.nc, K.P, K.I, K.S
    with contextlib.ExitStack() as st:
        sb = lambda n, s, d: st.enter_context(nc.sbuf_tensor(n, s, d))
        cc = sb("p0_cc", [2, D], F32)
        ccs = sb("p0_ccs", [2, D], F32)
        ccT = sb("p0_ccT", [128, 8, 2], F32)
        msb = sb("p0_m", [2, 6 * D], F32)
        bad = sb("p0_bad", [2, 6 * D], F32)
        wa = [sb(f"p0_wa{i}", [128, 8, 512], F32) for i in range(2)]
        pT = st.enter_context(nc.psum_tensor("p0_pT", [128, 8, 2], F32))
        pm = [st.enter_context(nc.psum_tensor(f"p0_pm{i}", [2, 512], F32)) for i in range(2)]
        b_cc, b_ccs, b_ccT, b_m, b_bad, b_pT = [Buf() for _ in range(6)]
        b_wa = [[Buf() for _ in range(8)] for _ in range(2)]
        b_pm = [Buf(), Buf()]
        P.dma("sync", cc[:], I["cc"], writes=[b_cc])
        P.dma("sync", bad[:], I["b_ada"].partition_broadcast(2), writes=[b_bad])
        P.op("act", lambda e: e.activation(out=ccs[:], in_=cc[:], func=AF.Silu), reads=[b_cc], writes=[b_ccs])
        for kc in range(8):
            P.op("pe", lambda e, kc=kc: e.transpose(pT[:, kc, :], ccs[:, kc * 128:(kc + 1) * 128], K.ident[0:2, 0:2]),
                 reads=[b_ccs, K.b_ident], writes=[b_pT])
        P.op("dve", lambda e: e.tensor_copy(ccT[:], pT[:]), reads=[b_pT], writes=[b_ccT])
        wav = I["w_ada"].rearrange("(kc p) n -> p kc n", p=128)
        for c in range(12):
            w = wa[c % 2]
            for kc in range(8):
                P.dma("sync", w[:, kc, :], wav[:, kc, c * 512:(c + 1) * 512], writes=[b_wa[c % 2][kc]])
            for kc in range(8):
                P.op("pe", lambda e, kc=kc, w=w, c=c: e.matmul(pm[c % 2][:], lhsT=ccT[:, kc, :], rhs=w[:, kc, :],
                                                              start=(kc == 0), stop=(kc == 7)),
                     reads=[b_ccT, b_wa[c % 2][kc]], writes=[b_pm[c % 2]])
            P.op("dve", lambda e, c=c: e.tensor_tensor(out=msb[:, c * 512:(c + 1) * 512], in0=pm[c % 2][:],
                                                       in1=bad[:, c * 512:(c + 1) * 512], op=ALU.add),
                 reads=[b_pm[c % 2], b_bad], writes=[b_m])
        P.dma("sync", S["M"], msb[:], reads=[b_m])
        P.wait_all_dma("sync")


def load_bcast(K, P, tile, dram_row, buf):
    return P.dma("sync", tile, dram_row.partition_broadcast(128), writes=[buf])


def phase1(K):
    nc, P, I, S = K.nc, K.P, K.I, K.S
    with contextlib.ExitStack() as st:
        sb = lambda n, s, d: st.enter_context(nc.sbuf_tensor(n, s, d))
        ps = lambda n, s, d: st.enter_context(nc.psum_tensor(n, s, d))
        Wb = sb("p1_W", [128, 8, INW], BF16)
        b_W = [[Buf() for _ in range(8)] for _ in range(13)]
        winv = I["w_in"].rearrange("(kc p) n -> p kc n", p=128)
        C1 = [2, 3, 4, 5, 7, 8]
        C2 = [0, 1, 6, 9, 10, 11, 12]

        def load_w(c):
            for kc in range(8):
                P.dma("pool", Wb[:, kc, c * 512:(c + 1) * 512], winv[:, kc, c * 512:(c + 1) * 512], writes=[b_W[c][kc]])

        g1 = [sb(f"p1_g1_{i}", [128, D], F32) for i in range(2)]
        sh = [sb(f"p1_sh_{i}", [128, D], F32) for i in range(2)]
        b_g1 = [Buf(), Buf()]
        b_sh = [Buf(), Buf()]
        hx = [sb(f"p1_hx{i}", [128, D], F32) for i in range(2)]
        b_hx = [Buf(), Buf()]
        n1 = hx[0]
        b_n1 = b_hx[0]
        load_bcast(K, P, n1[:], I["norm1_w"], b_n1)
        for r in range(2):
            load_bcast(K, P, g1[r][:], S["M"][r:r + 1, 1024:2048], b_g1[r])
            load_bcast(K, P, sh[r][:], S["M"][r:r + 1, 0:1024], b_sh[r])
            P.op("dve", lambda e, r=r: e.scalar_tensor_tensor(out=g1[r][:], in0=g1[r][:], scalar=1.0, in1=n1[:],
                                                             op0=ALU.add, op1=ALU.mult),
                 reads=[b_g1[r], b_n1], writes=[b_g1[r]])
        gains = {}
        b_gain = Buf()
        for n in ("q_norm_a", "k_norm_a", "q_norm_b", "k_norm_b"):
            gains[n] = sb("p1_" + n, [128, 64], F32)
            load_bcast(K, P, gains[n][:], I[n], b_gain)
        nhalf = sb("p1_nhalf", [128, 24], F32)
        b_nhalf = Buf()
        P.op("pool", lambda e: e.memset(nhalf[:], -0.5), writes=[b_nhalf])
        for c in C1:
            load_w(c)
        pending_w = list(C2)

        NXB, NRB = 3, 6
        xt = [sb(f"p1_xt{i}", [128, D], F32) for i in range(NXB)]
        b_xt = [Buf() for _ in range(NXB)]
        rc = [sb(f"p1_rc{i}", [128, 64], F32) for i in range(NRB)]
        rs_ = [sb(f"p1_rs{i}", [128, 64], F32) for i in range(NRB)]
        b_rope = [Buf() for _ in range(NRB)]
        junk = sb("p1_junk", [128, D], BF16)
        b_junk = Buf()
        ss = [sb(f"p1_ss{i}", [128, 1], F32) for i in range(2)]
        b_ss = [Buf(), Buf()]
        rstd = [sb(f"p1_rstd{i}", [128, 1], F32) for i in range(2)]
        b_rstd = [Buf(), Buf()]
        pT = ps("p1_pT", [128, 8, 128], F32)
        b_pT = Buf()
        hxT = [sb(f"p1_hxT{i}", [128, 8, 128], BF16) for i in range(2)]
        b_hxT = [Buf(), Buf()]
        NPC = 4
        pc = [ps(f"p1_pc{i}", [128, 512], F32) for i in range(NPC)]
        b_pc = [Buf() for _ in range(NPC)]
        pTb = [ps(f"p1_pTb{i}", [128, 8, 128], BF16) for i in range(2)]
        b_pTb = [Buf(), Buf()]
        T = [sb(f"p1_T{i}", [128, 24, 64], F32) for i in range(2)]
        b_T = [Buf(), Buf()]
        sq = sb("p1_sq", [128, 24, 64], F32)
        b_sq = Buf()
        ss8 = [sb(f"p1_ss8{i}", [128, 24], F32) for i in range(2)]
        b_ss8 = [Buf(), Buf()]
        r8 = [sb(f"p1_r8{i}", [128, 24], F32) for i in range(2)]
        b_r8 = [Buf(), Buf()]
        ra = sb("p1_ra", [128, 16, 64], F32)
        b_ra = Buf()
        rb = sb("p1_rb", [128, 16, 64], F32)
        b_rb = Buf()
        tn = [sb(f"p1_tn{i}", [128, 24 * 64], BF16) for i in range(2)]
        b_tn = [Buf(), Buf()]
        stA = [sb(f"p1_stA{i}", [128, 8, 256], BF16) for i in range(2)]
        stB = [sb(f"p1_stB{i}", [128, 4, 256], BF16) for i in range(2)]
        b_stA, b_stB = [Buf(), Buf()], [Buf(), Buf()]
        vg = [sb(f"p1_vg{i}", [128, 2048], BF16) for i in range(2)]
        b_vg = [Buf(), Buf()]

        seq = []
        for t in range(32):
            seq.append((1, t * 128, "A"))
        for t in range(32, 64):
            seq.append((1, t * 128, "B1" if t < 36 else "B"))
        for t in range(64, 66):
            seq.append((1, t * 128, "C"))
        for t in range(32):
            seq.append((2, t * 128, "Q"))
        N = len(seq)
        chunks_of = {"A": C1, "B1": C1, "C": C1, "B": [2, 3, 4, 5], "Q": C2}
        SLOT0 = {2: 0, 3: 8, 7: 16, 0: 0, 1: 8, 6: 16}
        cnt = {"pc": 0, "pTb": 0}

        def issue_load(i):
            if i >= N:
                return
            _, u0, _ = seq[i]
            j, jr = i % NXB, i % NRB
            P.dma("sync", xt[j][:], I["xall"][u0:u0 + 128, :], writes=[b_xt[j]])
            P.dma("sync", rc[jr][:], I["ropec"][u0:u0 + 128, :], writes=[b_rope[jr]])
            P.dma("sync", rs_[jr][:], I["ropes"][u0:u0 + 128, :], writes=[b_rope[jr]])

        def S1a(i):
            if i >= N:
                return
            _, u0, kind = seq[i]
            j, d = i % NXB, i % 2
            m = 1 if kind == "C" else 0
            P.op("act", lambda e: e.activation(out=junk[:], in_=xt[j][:], func=AF.Square, accum_out=ss[d][:]),
                 reads=[b_xt[j]], writes=[b_junk, b_ss[d]])
            P.op("dve", lambda e: e.tensor_scalar(out=ss[d][:], in0=ss[d][:], scalar1=1.0 / D, scalar2=EPS,
                                                  op0=ALU.mult, op1=ALU.add), reads=[b_ss[d]], writes=[b_ss[d]])
            P.op("pool", lambda e: e.tensor_tensor(out=rstd[d][:], in0=ss[d][:], in1=nhalf[:, 0:1], op=ALU.pow),
                 reads=[b_ss[d], b_nhalf], writes=[b_rstd[d]])
            P.op("dve", lambda e: e.scalar_tensor_tensor(out=hx[d][:], in0=xt[j][:], scalar=rstd[d][:, 0:1],
                                                         in1=g1[m][:], op0=ALU.mult, op1=ALU.mult),
                 reads=[b_xt[j], b_rstd[d], b_g1[m]], writes=[b_hx[d]])
            P.op("pool", lambda e: e.tensor_tensor(out=hx[d][:], in0=hx[d][:], in1=sh[m][:], op=ALU.add),
                 reads=[b_hx[d], b_sh[m]], writes=[b_hx[d]])

        def S1b(i):
            if i >= N:
                return
            d = i % 2
            for kc in range(8):
                P.op("pe", lambda e, kc=kc: e.transpose(pT[:, kc, :], hx[d][:, kc * 128:(kc + 1) * 128], K.ident[:]),
                     reads=[b_hx[d], K.b_ident], writes=[b_pT], inc=(kc == 7))
            P.op("act", lambda e: e.copy(out=hxT[d][:], in_=pT[:]), reads=[b_pT], writes=[b_hxT[d]])

        def S2(i):
            pas, u0, kind = seq[i]
            d = i % 2
            for c in chunks_of[kind]:
                pi = cnt["pc"] % NPC
                cnt["pc"] += 1
                for kc in range(8):
                    P.op("pe", lambda e, kc=kc, c=c, pi=pi: e.matmul(pc[pi][:], lhsT=hxT[d][:, kc, :],
                                                                    rhs=Wb[:, kc, c * 512:(c + 1) * 512],
                                                                    start=(kc == 0), stop=(kc == 7)),
                         reads=[b_hxT[d], b_W[c][kc]], writes=[b_pc[pi]], inc=(kc == 7))
                if c in SLOT0:
                    s0 = SLOT0[c]
                    P.op("act", lambda e, pi=pi, s0=s0: e.copy(out=T[d][:, s0:s0 + 8, :].rearrange("p a b -> p (a b)"), in_=pc[pi][:]),
                         reads=[b_pc[pi]], writes=[b_T[d]])
                elif c in (4, 5, 8):
                    off = {4: 0, 5: 512, 8: 1024}[c]
                    P.op("act", lambda e, pi=pi, off=off: e.copy(out=vg[d][:, off:off + 512], in_=pc[pi][:]),
                         reads=[b_pc[pi]], writes=[b_vg[d]])
                else:
                    off = (c - 9) * 512
                    P.op("act", lambda e, pi=pi, off=off: e.activation(out=vg[d][:, off:off + 512], in_=pc[pi][:], func=AF.Sigmoid),
                         reads=[b_pc[pi]], writes=[b_vg[d]])
            if pas == 1:
                P.dma("sync", S["VA"][u0:u0 + 128, :], vg[d][:, 0:1024], reads=[b_vg[d]])
                if kind != "B":
                    ub = u0 if kind != "C" else 4608 + (u0 - NT)
                    P.dma("sync", S["VB"][ub:ub + 128, :], vg[d][:, 1024:1536], reads=[b_vg[d]])
            else:
                P.dma("sync", S["GA"][u0:u0 + 128, :], vg[d][:, 0:1024], reads=[b_vg[d]])
                P.dma("sync", S["GB"][u0:u0 + 128, :], vg[d][:, 1024:2048], reads=[b_vg[d]])

        def S3(i):
            pas, u0, kind = seq[i]
            d, jr = i % 2, i % NRB
            n = 16 if kind == "B" else 24
            gA = gains["k_norm_a" if pas == 1 else "q_norm_a"]
            gB = gains["k_norm_b" if pas == 1 else "q_norm_b"]
            Tt = T[d]
            tn3 = tn[d][:].rearrange("p (a b) -> p a b", b=64)
            P.op("dve", lambda e: e.tensor_tensor(out=sq[:, 0:n, :], in0=Tt[:, 0:n, :], in1=Tt[:, 0:n, :], op=ALU.mult),
                 reads=[b_T[d]], writes=[b_sq])
            P.op("dve", lambda e: e.tensor_reduce(out=ss8[d][:, 0:n], in_=sq[:, 0:n, :], axis=AX.X, op=ALU.add),
                 reads=[b_sq], writes=[b_ss8[d]])
            P.op("dve", lambda e: e.tensor_scalar(out=ss8[d][:, 0:n], in0=ss8[d][:, 0:n], scalar1=1.0 / 64, scalar2=EPS,
                                                  op0=ALU.mult, op1=ALU.add), reads=[b_ss8[d]], writes=[b_ss8[d]])
            P.op("pool", lambda e: e.tensor_tensor(out=r8[d][:, 0:n], in0=ss8[d][:, 0:n], in1=nhalf[:, 0:n], op=ALU.pow),
                 reads=[b_ss8[d], b_nhalf], writes=[b_r8[d]])
            P.op("dve", lambda e: e.tensor_tensor(out=Tt[:, 0:n, :], in0=Tt[:, 0:n, :],
                                                  in1=r8[d][:, 0:n].unsqueeze(2).to_broadcast([128, n, 64]), op=ALU.mult),
                 reads=[b_T[d], b_r8[d]], writes=[b_T[d]])
            P.op("dve", lambda e: e.tensor_tensor(out=Tt[:, 0:16, :], in0=Tt[:, 0:16, :],
                                                  in1=gA[:].unsqueeze(1).to_broadcast([128, 16, 64]), op=ALU.mult),
                 reads=[b_T[d], b_gain], writes=[b_T[d]])
            if n == 24:
                P.op("dve", lambda e: e.tensor_tensor(out=tn3[:, 16:24, :], in0=Tt[:, 16:24, :],
                                                      in1=gB[:].unsqueeze(1).to_broadcast([128, 8, 64]), op=ALU.mult),
                     reads=[b_T[d], b_gain], writes=[b_tn[d]])
            P.op("dve", lambda e: e.tensor_tensor(out=ra[:], in0=Tt[:, 0:16, :],
                                                  in1=rc[jr][:].unsqueeze(1).to_broadcast([128, 16, 64]), op=ALU.mult),
                 reads=[b_T[d], b_rope[jr]], writes=[b_ra])
            t5 = Tt[:, 0:16, :].rearrange("p a (x y f) -> p a x y f", x=2, y=2)
            rb5 = rb[:].rearrange("p a (x y f) -> p a x y f", x=2, y=2)
            s5 = rs_[jr][:].rearrange("p (x y f) -> p x y f", x=2, y=2)
            for a in range(2):
                for y in range(2):
                    P.op("pool", lambda e, a=a, y=y: e.tensor_tensor(
                        out=rb5[:, :, a, y, :], in0=t5[:, :, a, 1 - y, :],
                        in1=s5[:, a, y, :].unsqueeze(1).to_broadcast([128, 16, 16]), op=ALU.mult),
                         reads=[b_T[d], b_rope[jr]], writes=[b_rb])
            P.op("dve", lambda e: e.tensor_tensor(out=tn3[:, 0:16, :], in0=ra[:], in1=rb[:], op=ALU.add),
                 reads=[b_ra, b_rb], writes=[b_tn[d]])

        def S4(i):
            if i < 0:
                return
            pas, u0, kind = seq[i]
            d, gi, tg = i % 2, (i // 2) % 2, i % 2
            qi = cnt["pTb"] % 2
            cnt["pTb"] += 1
            for jj in range(8):
                P.op("pe", lambda e, jj=jj: e.transpose(pTb[qi][:, jj, :], tn[d][:, jj * 128:(jj + 1) * 128], K.identb[:]),
                     reads=[b_tn[d], K.b_identb], writes=[b_pTb[qi]], inc=(jj == 7))
            P.op("act", lambda e: e.copy(out=stA[gi][:, :, tg * 128:(tg + 1) * 128], in_=pTb[qi][:]),
                 reads=[b_pTb[qi]], writes=[b_stA[gi]])
            if kind != "B":
                q2 = cnt["pTb"] % 2
                cnt["pTb"] += 1
                for jj in range(4):
                    P.op("pe", lambda e, jj=jj: e.transpose(pTb[q2][:, jj, :], tn[d][:, (8 + jj) * 128:(9 + jj) * 128], K.identb[:]),
                         reads=[b_tn[d], K.b_identb], writes=[b_pTb[q2]], inc=(jj == 3))
                P.op("act", lambda e: e.copy(out=stB[gi][:, :, tg * 128:(tg + 1) * 128], in_=pTb[q2][:, 0:4, :]),
                     reads=[b_pTb[q2]], writes=[b_stB[gi]])
            if tg == 1:
                g0 = u0 - 128
                if pas == 1:
                    P.dma("sync", S["KTA"][:, :, g0:g0 + 256].rearrange("h p t -> p h t"), stA[gi][:], reads=[b_stA[gi]])
                    if kind != "B":
                        gb0 = g0 if kind != "C" else 4608 + (g0 - NT)
                        P.dma("sync", S["KTB"][:, :, gb0:gb0 + 256].rearrange("h p t -> p h t"), stB[gi][:], reads=[b_stB[gi]])
                else:
                    P.dma("sync", S["QTA"][:, :, g0:g0 + 256].rearrange("h p t -> p h t"), stA[gi][:], reads=[b_stA[gi]])
                    P.dma("sync", S["QTB"][:, :, g0:g0 + 256].rearrange("h p t -> p h t"), stB[gi][:], reads=[b_stB[gi]])

        issue_load(0)
        issue_load(1)
        issue_load(2)
        S1a(0)
        S1a(1)
        S1b(0)
        for i in range(N):
            issue_load(i + 3)
            if pending_w and i % 4 == 1:
                load_w(pending_w.pop(0))
            S4(i - 2)
            S1b(i + 1)
            S2(i)
            if i >= 1:
                S3(i - 1)
            S1a(i + 2)
        S3(N - 1)
        S4(N - 2)
        S4(N - 1)
        P.wait_all_dma("sync")


LAM_INIT = 0.2


def phase2(K, heads=range(8), qblocks=range(8), precast=True):
    nc, P, I, S = K.nc, K.P, K.I, K.S
    NKT = NKA // 128
    with contextlib.ExitStack() as st:
        sb = lambda n, s, d: st.enter_context(nc.sbuf_tensor(n, s, d))
        ps = lambda n, s, d: st.enter_context(nc.psum_tensor(n, s, d))
        KT = [sb(f"p2_KT{i}", [128, NKA], BF16) for i in range(2)]
        QT = [sb(f"p2_QT{i}", [128, NOWN], BF16) for i in range(2)]
        V = [sb(f"p2_V{i}", [128, NKT, 130], BF16) for i in range(2)]
        b_KT, b_QT, b_V = [Buf(), Buf()], [Buf(), Buf()], [Buf(), Buf()]
        for i in range(2):
            P.op("pool", lambda e, i=i: e.memset(V[i][:, :, 128:130], 1.0), writes=[b_V[i]])
        lv = sb("p2_lv", [128, 4, 64], F32)
        b_lv = Buf()
        for i, n in enumerate(("lambda_q1", "lambda_k1", "lambda_q2", "lambda_k2")):
            load_bcast(K, P, lv[:, i, :], I[n], b_lv)
        lprod = sb("p2_lprod", [128, 2, 64], F32)
        lsum = sb("p2_lsum", [128, 2], F32)
        lam = sb("p2_lam", [128, 1], F32)
        b_lam = Buf()
        P.op("dve", lambda e: e.tensor_tensor(out=lprod[:, 0, :], in0=lv[:, 0, :], in1=lv[:, 1, :], op=ALU.mult), reads=[b_lv], writes=[b_lam])
        P.op("dve", lambda e: e.tensor_tensor(out=lprod[:, 1, :], in0=lv[:, 2, :], in1=lv[:, 3, :], op=ALU.mult), reads=[b_lv], writes=[b_lam])
        P.op("dve", lambda e: e.tensor_reduce(out=lsum[:], in_=lprod[:], axis=AX.X, op=ALU.add), reads=[b_lam], writes=[b_lam])
        P.op("act", lambda e: e.activation(out=lsum[:], in_=lsum[:], func=AF.Exp), reads=[b_lam], writes=[b_lam])
        P.op("dve", lambda e: e.tensor_tensor(out=lam[:], in0=lsum[:, 0:1], in1=lsum[:, 1:2], op=ALU.subtract), reads=[b_lam], writes=[b_lam])
        P.op("dve", lambda e: e.tensor_scalar(out=lam[:], in0=lam[:], scalar1=LAM_INIT, scalar2=None, op0=ALU.add), reads=[b_lam], writes=[b_lam])
        gsub = sb("p2_gsub", [128, 128], F32)
        b_gsub = Buf()
        load_bcast(K, P, gsub[:], I["subln_a"], b_gsub)
        P.op("dve", lambda e: e.tensor_scalar(out=gsub[:], in0=gsub[:], scalar1=1.0 - LAM_INIT, scalar2=None, op0=ALU.mult),
             reads=[b_gsub], writes=[b_gsub])
        nhalf = sb("p2_nhalf", [128, 1], F32)
        b_nhalf = Buf()
        P.op("pool", lambda e: e.memset(nhalf[:], -0.5), writes=[b_nhalf])

        pS = [ps(f"p2_pS{i}", [128, 1024], F32) for i in range(2)]
        b_pS = [Buf(), Buf()]
        pO = [ps(f"p2_pO{i}", [128, 512], F32) for i in range(3)]
        b_pO = Buf()
        pTr = ps("p2_pTr", [128, 8, 128], BF16)
        b_pTr = Buf()
        NPT = 3
        pt = [sb(f"p2_pt{i}", [128, 1024], BF16) for i in range(NPT)]
        b_pt = [Buf() for _ in range(NPT)]
        def acc(j):
            return pO[j // 3][:, (j % 3) * 129:(j % 3) * 129 + 129]
        osb = [sb(f"p2_osb{i}", [128, 8, 129], F32) for i in range(2)]
        b_osb = [Buf(), Buf()]
        rz = sb("p2_rz", [128, 8], F32)
        b_rz = Buf()
        o1s = sb("p2_o1s", [128, 4, 128], F32)
        o2s = sb("p2_o2s", [128, 4, 128], F32)
        osq = sb("p2_osq", [128, 4, 128], F32)
        b_f = Buf()
        oss = sb("p2_oss", [128, 4], F32)
        ors = sb("p2_ors", [128, 4], F32)
        b_oss, b_ors = Buf(), Buf()
        nhalf4 = sb("p2_nhalf4", [128, 4], F32)
        P.op("pool", lambda e: e.memset(nhalf4[:], -0.5), writes=[b_nhalf])
        onb = [sb(f"p2_on{i}", [128, 4, 128], BF16) for i in range(2)]
        b_on = [Buf(), Buf()]
        oT = [sb(f"p2_oT{i}", [128, 512], BF16) for i in range(2)]
        b_oT = [Buf(), Buf()]

        def load_head(h, par):
            P.dma("sync", KT[par][:], S["KTA"][h], writes=[b_KT[par]])
            P.dma("sync", QT[par][:], S["QTA"][h], writes=[b_QT[par]])
            vv = S["VA"][:, h * 128:(h + 1) * 128].rearrange("(kt p) d -> p kt d", p=128)
            for k0 in range(0, NKT, 22):
                P.dma("sync", V[par][:, k0:k0 + 22, 0:128], vv[:, k0:k0 + 22, :], writes=[b_V[par]])

        heads = list(heads)
        qblocks = list(qblocks)
        its = [(hi, h, qb, kt) for hi, h in enumerate(heads) for qb in qblocks for kt in range(NKT)]

        def emit_S(n):
            if n >= len(its):
                return
            hi, h, qb, kt = its[n]
            par = hi % 2
            sp = n % 2
            P.op("pe", lambda e: e.matmul(pS[sp][:, 0:512], lhsT=KT[par][0:64, kt * 128:(kt + 1) * 128],
                                          rhs=QT[par][0:64, qb * 512:(qb + 1) * 512], start=True, stop=True, tile_position=(0, 0)),
                 reads=[b_KT[par], b_QT[par]], writes=[b_pS[sp]], inc=False)
            P.op("pe", lambda e: e.matmul(pS[sp][:, 512:1024], lhsT=KT[par][64:128, kt * 128:(kt + 1) * 128],
                                          rhs=QT[par][64:128, qb * 512:(qb + 1) * 512], start=True, stop=True, tile_position=(64, 0)),
                 reads=[b_KT[par], b_QT[par]], writes=[b_pS[sp]])

        def fin_part1(oi):
            ob3 = osb[oi]
            P.op("dve", lambda e: e.tensor_copy(ob3[:, 0:3, :], pO[0][:, 0:387].rearrange("p (a b) -> p a b", b=129)), reads=[b_pO], writes=[b_osb[oi]])
            P.op("dve", lambda e: e.tensor_copy(ob3[:, 3:6, :], pO[1][:, 0:387].rearrange("p (a b) -> p a b", b=129)), reads=[b_pO], writes=[b_osb[oi]])
            P.op("dve", lambda e: e.tensor_copy(ob3[:, 6:8, :], pO[2][:, 0:258].rearrange("p (a b) -> p a b", b=129)), reads=[b_pO], writes=[b_osb[oi]])
            P.op("dve", lambda e: e.reciprocal(out=rz[:].unsqueeze(2), in_=ob3[:, :, 128:129]), reads=[b_osb[oi]], writes=[b_rz])
            P.op("dve", lambda e: e.tensor_scalar(out=rz[:, 4:8], in0=rz[:, 4:8], scalar1=lam[:, 0:1], scalar2=None, op0=ALU.mult),
                 reads=[b_rz, b_lam], writes=[b_rz])
            P.op("dve", lambda e: e.tensor_tensor(out=o2s[:], in0=ob3[:, 4:8, 0:128], in1=rz[:, 4:8].unsqueeze(2).to_broadcast([128, 4, 128]), op=ALU.mult),
                 reads=[b_osb[oi], b_rz], writes=[b_f])
            P.op("dve", lambda e: e.tensor_tensor(out=o1s[:], in0=ob3[:, 0:4, 0:128], in1=rz[:, 0:4].unsqueeze(2).to_broadcast([128, 4, 128]), op=ALU.mult),
                 reads=[b_osb[oi], b_rz], writes=[b_f])
            P.op("dve", lambda e: e.tensor_tensor(out=o1s[:], in0=o1s[:], in1=o2s[:], op=ALU.subtract), reads=[b_f], writes=[b_f])
            P.op("dve", lambda e: e.tensor_tensor(out=osq[:], in0=o1s[:], in1=o1s[:], op=ALU.mult), reads=[b_f], writes=[b_f])
            P.op("dve", lambda e: e.tensor_reduce(out=oss[:], in_=osq[:], axis=AX.X, op=ALU.add), reads=[b_f], writes=[b_oss])
            P.op("dve", lambda e: e.tensor_scalar(out=oss[:], in0=oss[:], scalar1=1.0 / 128, scalar2=EPS, op0=ALU.mult, op1=ALU.add),
                 reads=[b_oss], writes=[b_oss])
            P.op("pool", lambda e: e.tensor_tensor(out=ors[:], in0=oss[:], in1=nhalf4[:], op=ALU.pow), reads=[b_oss, b_nhalf], writes=[b_ors])
            P.op("dve", lambda e: e.tensor_tensor(out=o1s[:], in0=o1s[:], in1=ors[:].unsqueeze(2).to_broadcast([128, 4, 128]), op=ALU.mult),
                 reads=[b_f, b_ors], writes=[b_f])
            P.op("dve", lambda e: e.tensor_tensor(out=onb[oi][:], in0=o1s[:], in1=gsub[:].unsqueeze(1).to_broadcast([128, 4, 128]), op=ALU.mult),
                 reads=[b_f, b_gsub], writes=[b_on[oi]])

        def fin_part2(h, qb, oi):
            for qi in range(4):
                P.op("pe", lambda e, qi=qi: e.transpose(pTr[:, qi, :], onb[oi][:, qi, :], K.identb[:]),
                     reads=[b_on[oi], K.b_identb], writes=[b_pTr], inc=(qi == 3))
            P.op("dve", lambda e: e.tensor_copy(oT[oi][:].rearrange("p (a b) -> p a b", b=128), pTr[:, 0:4, :]),
                 reads=[b_pTr], writes=[b_oT[oi]])
            P.dma("sync", S["OAT"][h][:, qb * 512:(qb + 1) * 512], oT[oi][:], reads=[b_oT[oi]])

        cst = {m: [sb(f"p2_cst_{m}{i}", [128, 4096], BF16) for i in range(2)] for m in "gud"}
        b_cst = {m: [[Buf() for _ in range(8)] for _ in range(2)] for m in "gud"}
        units = [(e, m) for e in range(32) for m in "gud"] if precast else []

        def emit_cast(u):
            e, m = units[u]
            par = e % 2
            if m == "d":
                v = I["w_expert_down"][e].rearrange("(c p) n -> p c n", p=128)
                t3 = cst[m][par][:].rearrange("p (a b) -> p a b", b=D)
                nch, dst = 4, S["WD16"]
            else:
                v = (I["w_expert_gate"] if m == "g" else I["w_expert_up"])[e].rearrange("(c p) n -> p c n", p=128)
                t3 = cst[m][par][:].rearrange("p (a b) -> p a b", b=512)
                nch, dst = 8, (S["WG16"] if m == "g" else S["WU16"])
            for c in range(nch):
                P.dma("pool", t3[:, c, :], v[:, c, :], writes=[b_cst[m][par][c]])
            P.dma("sync", dst[e * 128:(e + 1) * 128, :], cst[m][par][:], reads=b_cst[m][par][0:nch])

        load_head(heads[0], 0)
        emit_S(0)
        emit_S(1)
        fin = 0
        pending = None
        ucast = 0
        for n, (hi, h, qb, kt) in enumerate(its):
            par = hi % 2
            if qb == qblocks[0] and kt == 0 and hi + 1 < len(heads):
                load_head(heads[hi + 1], (hi + 1) % 2)
            sp = n % 2
            pi = n % NPT
            P.op("act", lambda e, sp=sp, pi=pi: e.activation(out=pt[pi][:], in_=pS[sp][:], func=AF.Exp, scale=0.125),
                 reads=[b_pS[sp]], writes=[b_pt[pi]])
            emit_S(n + 2)
            for j in range(8):
                half, qi = j // 4, j % 4
                P.op("pe", lambda e, j=j, half=half, qi=qi, pi=pi, par=par, kt=kt: e.matmul(
                    acc(j), lhsT=pt[pi][:, half * 512 + qi * 128: half * 512 + (qi + 1) * 128], rhs=V[par][:, kt, 0:129],
                    start=(kt == 0 and j % 3 == 0), stop=(kt == NKT - 1), skip_group_check=True),
                     reads=[b_pt[pi], b_V[par]], writes=[b_pO], inc=(j == 7))
            if kt == 20 and pending is not None:
                fin_part2(*pending)
                pending = None
            if n % 43 == 5 and ucast < len(units):
                emit_cast(ucast)
                ucast += 1
            if kt == NKT - 1:
                if pending is not None:
                    fin_part2(*pending)
                oi = fin % 2
                fin += 1
                fin_part1(oi)
                pending = (h, qb, oi)
        if pending is not None:
            fin_part2(*pending)
        while ucast < len(units):
            emit_cast(ucast)
            ucast += 1
        P.wait_all_dma("sync")


def phase3(K, pairs=range(4), slots=range(8)):
    nc, P, I, S = K.nc, K.P, K.I, K.S
    NKT = NKB // 128
    with contextlib.ExitStack() as st:
        sb = lambda n, s, d: st.enter_context(nc.sbuf_tensor(n, s, d))
        ps = lambda n, s, d: st.enter_context(nc.psum_tensor(n, s, d))
        KT2 = [sb(f"p3_KT{i}", [128, NKB], BF16) for i in range(2)]
        QT2 = [sb(f"p3_QT{i}", [128, NOWN], BF16) for i in range(2)]
        V2 = [sb(f"p3_V{i}", [128, NKT, 2, 66], BF16) for i in range(2)]
        b_KT2, b_QT2 = [Buf(), Buf()], [Buf(), Buf()]
        b_V2 = [[Buf(), Buf()] for _ in range(2)]
        for i in range(2):
            P.op("pool", lambda e, i=i: e.memset(V2[i][:, :, :, 64:66], 1.0), writes=b_V2[i])
        EB2 = [sb(f"p3_EB{i}", [128, 2, 8, 2, 512], BF16) for i in range(2)]
        b_EB2 = [Buf(), Buf()]
        bstg = [sb(f"p3_bstg{i}", [128, 512], F32) for i in range(8)]
        b_bstg = [Buf() for _ in range(8)]
        pairs = list(pairs)
        bunits = [(v, t, hh) for v in range(2) for t in range(8) for hh in range(2)]

        def load_pair(pi_):
            j, pp = pairs[pi_], pi_ % 2
            P.dma("sync", KT2[pp][:], S["KTB"][j], writes=[b_KT2[pp]])
            P.dma("sync", QT2[pp][:], S["QTB"][j], writes=[b_QT2[pp]])
            vv = S["VB"][:, j * 128:(j + 1) * 128].rearrange("(kt p) (hh d) -> p kt hh d", p=128, hh=2)
            for hh in range(2):
                P.dma("sync", V2[pp][:, :, hh, 0:64], vv[:, :, hh, :], writes=[b_V2[pp][hh]])

        def bias_dma(pi_, g):
            j = pairs[pi_]
            for q in range(4):
                v, t, hh = bunits[g * 4 + q]
                bi = (g % 2) * 4 + q
                P.dma("sync", bstg[bi][:], I["nabias"][v, 2 * j + hh, t], writes=[b_bstg[bi]])

        def bias_exp(pi_, g):
            pp = pi_ % 2
            for q in range(4):
                v, t, hh = bunits[g * 4 + q]
                bi = (g % 2) * 4 + q
                P.op("act", lambda e, v=v, t=t, hh=hh, bi=bi: e.activation(out=EB2[pp][:, v, t, hh, :], in_=bstg[bi][:], func=AF.Exp),
                     reads=[b_bstg[bi]], writes=[b_EB2[pp]])
        pS = [ps(f"p3_pS{i}", [128, 1024], F32) for i in range(2)]
        b_pS = [Buf(), Buf()]
        pO = [ps(f"p3_pO{i}", [128, 512], F32) for i in range(2)]
        b_pO = Buf()
        pTr = ps("p3_pTr", [128, 8, 128], BF16)
        b_pTr = Buf()
        NPT = 3
        et = [sb(f"p3_et{i}", [128, 1024], BF16) for i in range(NPT)]
        b_et = [Buf() for _ in range(NPT)]
        pm = [sb(f"p3_pm{i}", [128, 1024], BF16) for i in range(NPT)]
        b_pm = [Buf() for _ in range(NPT)]
        rz = sb("p3_rz", [128, 8], F32)
        b_rz = Buf()
        onb = [sb(f"p3_on{i}", [128, 4, 128], BF16) for i in range(2)]
        b_on = [Buf(), Buf()]
        oT = [sb(f"p3_oT{i}", [128, 512], BF16) for i in range(2)]
        b_oT = [Buf(), Buf()]

        def acc(hh, qi):
            return pO[hh][:, qi * 65:qi * 65 + 65]

        fin = 0
        n = 0
        load_pair(0)
        bias_dma(0, 0)
        for g in range(8):
            if g + 1 < 8:
                bias_dma(0, g + 1)
            bias_exp(0, g)
        osb = [sb(f"p3_osb{i}", [128, 2, 260], F32) for i in range(2)]
        b_osb = [Buf(), Buf()]
        slots = list(slots)
        its3 = [(pi_, si, idx) for pi_ in range(len(pairs)) for si in range(len(slots)) for idx in range(10)]

        def ktis_of(s):
            KR0 = min(max(8 * s - 4, 0), 112)
            return [KR0 // 2 + t for t in range(8)] + [36, 37]

        def emit_S(m):
            if m >= len(its3):
                return
            pi_, si, idx = its3[m]
            pp, s = pi_ % 2, slots[si]
            kti = ktis_of(s)[idx]
            sp = m % 2
            KT, QT = KT2[pp], QT2[pp]
            P.op("pe", lambda e: e.matmul(pS[sp][:, 0:512], lhsT=KT[0:64, kti * 128:(kti + 1) * 128],
                                          rhs=QT[0:64, s * 512:(s + 1) * 512], start=True, stop=True, tile_position=(0, 0)),
                 reads=[b_KT2[pp], b_QT2[pp]], writes=[b_pS[sp]], inc=False)
            P.op("pe", lambda e: e.matmul(pS[sp][:, 512:1024], lhsT=KT[64:128, kti * 128:(kti + 1) * 128],
                                          rhs=QT[64:128, s * 512:(s + 1) * 512], start=True, stop=True, tile_position=(64, 0)),
                 reads=[b_KT2[pp], b_QT2[pp]], writes=[b_pS[sp]])

        def fin_part1(oi):
            for hh in range(2):
                P.op("dve", lambda e, hh=hh: e.tensor_copy(osb[oi][:, hh, :], pO[hh][:, 0:260]), reads=[b_pO], writes=[b_osb[oi]])
            o4 = osb[oi][:].rearrange("p h (q c) -> p (h q) c", c=65)
            P.op("dve", lambda e: e.reciprocal(out=rz[:].unsqueeze(2), in_=o4[:, :, 64:65]), reads=[b_osb[oi]], writes=[b_rz])
            for hh in range(2):
                P.op("dve", lambda e, hh=hh: e.tensor_tensor(out=onb[oi][:, :, hh * 64:(hh + 1) * 64], in0=o4[:, hh * 4:(hh + 1) * 4, 0:64],
                                                             in1=rz[:, hh * 4:(hh + 1) * 4].unsqueeze(2).to_broadcast([128, 4, 64]), op=ALU.mult),
                     reads=[b_osb[oi], b_rz], writes=[b_on[oi]])

        def fin_part2(j, s, oi):
            for qi in range(4):
                P.op("pe", lambda e, qi=qi: e.transpose(pTr[:, qi, :], onb[oi][:, qi, :], K.identb[:]),
                     reads=[b_on[oi], K.b_identb], writes=[b_pTr], inc=(qi == 3))
            P.op("dve", lambda e: e.tensor_copy(oT[oi][:].rearrange("p (a b) -> p a b", b=128), pTr[:, 0:4, :]),
                 reads=[b_pTr], writes=[b_oT[oi]])
            P.dma("sync", S["OBT"][j][:, s * 512:(s + 1) * 512], oT[oi][:], reads=[b_oT[oi]])

        emit_S(0)
        emit_S(1)
        pending = None
        gd = ge = 8
        for m, (pi_, si, idx) in enumerate(its3):
            j, pp, s = pairs[pi_], pi_ % 2, slots[si]
            V, EB = V2[pp], EB2[pp]
            nxt = pi_ + 1 if pi_ + 1 < len(pairs) else None
            if idx == 0:
                if si == 0:
                    if pi_ > 0:
                        while ge < 8:
                            if gd < 8:
                                bias_dma(pi_, gd)
                                gd += 1
                            bias_exp(pi_, ge)
                            ge += 1
                    gd = ge = 0 if nxt is not None else 8
                    if nxt is not None:
                        load_pair(nxt)
                if nxt is not None:
                    if gd < 8:
                        bias_dma(nxt, gd)
                        gd += 1
                    if ge < gd - 1:
                        bias_exp(nxt, ge)
                        ge += 1
            v = 0 if s == 0 else 1
            kti = ktis_of(s)[idx]
            sp, pi = m % 2, m % NPT
            P.op("act", lambda e, sp=sp, pi=pi: e.activation(out=et[pi][:], in_=pS[sp][:], func=AF.Exp, scale=0.125),
                 reads=[b_pS[sp]], writes=[b_et[pi]])
            emit_S(m + 2)
            if idx < 8:
                P.op("dve", lambda e, pi=pi, v=v, idx=idx, EB=EB: e.tensor_tensor(out=pm[pi][:], in0=et[pi][:],
                                                                                  in1=EB[:, v, idx, :, :].rearrange("p a b -> p (a b)"), op=ALU.mult),
                     reads=[b_et[pi], b_EB2[pp]], writes=[b_pm[pi]])
                src_t, b_src = pm[pi], b_pm[pi]
            else:
                src_t, b_src = et[pi], b_et[pi]
            for hh in range(2):
                for qi in range(4):
                    P.op("pe", lambda e, hh=hh, qi=qi, src_t=src_t, kti=kti, idx=idx, V=V: e.matmul(
                        acc(hh, qi), lhsT=src_t[:, hh * 512 + qi * 128: hh * 512 + (qi + 1) * 128], rhs=V[:, kti, hh, 0:65],
                        start=(idx == 0 and qi == 0), stop=(idx == 9), skip_group_check=True),
                         reads=[b_src] + b_V2[pp], writes=[b_pO], inc=(hh == 1 and qi == 3))
            if idx == 4 and pending is not None:
                fin_part2(*pending)
                pending = None
            if idx == 9:
                if pending is not None:
                    fin_part2(*pending)
                oi = fin % 2
                fin += 1
                fin_part1(oi)
                pending = (j, s, oi)
        if pending is not None:
            fin_part2(*pending)
        P.wait_all_dma("sync")


BIG = 1.0e4


def phase4a(K, tiles=range(32), stop=99):
    nc, P, I, S = K.nc, K.P, K.I, K.S
    with contextlib.ExitStack() as st:
        sb = lambda n, s, d: st.enter_context(nc.sbuf_tensor(n, s, d))
        ps = lambda n, s, d: st.enter_context(nc.psum_tensor(n, s, d))
        Wa = sb("p4_Wa", [128, 8, D], BF16)
        Wbb = sb("p4_Wb", [128, 4, D], BF16)
        Wo = sb("p4_Wo", [128, 8, D], BF16)
        Wr = sb("p4_Wr", [128, 8, 36], F32)
        b_Wa = [[Buf(), Buf()] for _ in range(8)]
        b_Wb = [[Buf(), Buf()] for _ in range(4)]
        b_Wo = [[Buf(), Buf()] for _ in range(8)]
        b_Wr = Buf()
        wav = I["w_branch_a"].rearrange("(kc p) n -> p kc n", p=128)
        wbv = I["w_branch_b"].rearrange("(kc p) n -> p kc n", p=128)
        wov = I["w_out"].rearrange("(kc p) n -> p kc n", p=128)
        for kc in range(8):
            for c in range(2):
                P.dma("pool", Wa[:, kc, c * 512:(c + 1) * 512], wav[:, kc, c * 512:(c + 1) * 512], writes=[b_Wa[kc][c]])
        for kc in range(4):
            for c in range(2):
                P.dma("pool", Wbb[:, kc, c * 512:(c + 1) * 512], wbv[:, kc, c * 512:(c + 1) * 512], writes=[b_Wb[kc][c]])
        for kc in range(8):
            for c in range(2):
                P.dma("pool", Wo[:, kc, c * 512:(c + 1) * 512], wov[:, kc, c * 512:(c + 1) * 512], writes=[b_Wo[kc][c]])
        P.dma("sync", Wr[:], I["w_router"].rearrange("(kc p) n -> p kc n", p=128), writes=[b_Wr])
        ga1 = sb("p4_ga1", [128, D], F32)
        g2 = sb("p4_g2", [128, D], F32)
        sh2 = sb("p4_sh2", [128, D], F32)
        brt = sb("p4_brt", [128, 36], F32)
        b_ga1, b_g2, b_sh2, b_brt, b_c = Buf(), Buf(), Buf(), Buf(), Buf()
        xm = [sb(f"p4_xm{i}", [128, D], F32) for i in range(2)]
        b_xm = [Buf(), Buf()]
        load_bcast(K, P, ga1[:], S["M"][0:1, 2048:3072], b_ga1)
        load_bcast(K, P, sh2[:], S["M"][0:1, 3072:4096], b_sh2)
        load_bcast(K, P, g2[:], S["M"][0:1, 4096:5120], b_g2)
        load_bcast(K, P, xm[0][:], I["norm2_w"], b_xm[0])
        load_bcast(K, P, brt[:], I["b_router"], b_brt)
        P.op("dve", lambda e: e.scalar_tensor_tensor(out=g2[:], in0=g2[:], scalar=1.0, in1=xm[0][:], op0=ALU.add, op1=ALU.mult),
             reads=[b_g2, b_xm[0]], writes=[b_g2])
        nhalf = sb("p4_nhalf", [128, 1], F32)
        P.op("pool", lambda e: e.memset(nhalf[:], -0.5), writes=[b_c])

        NB = 4
        oaT = [sb(f"p4_oaT{i}", [128, 8, 128], BF16) for i in range(NB)]
        obT = [sb(f"p4_obT{i}", [128, 4, 128], BF16) for i in range(NB)]
        gat = [sb(f"p4_gat{i}", [128, D], BF16) for i in range(NB)]
        gbt = [sb(f"p4_gbt{i}", [128, D], BF16) for i in range(NB)]
        xt = [sb(f"p4_xt{i}", [128, D], F32) for i in range(NB)]
        b_oa, b_ob, b_ga, b_gb, b_xt = [[Buf() for _ in range(NB)] for _ in range(5)]
        t1 = sb("p4_t1", [128, D], F32)
        t2 = sb("p4_t2", [128, D], F32)
        t3 = sb("p4_t3", [128, D], F32)
        b_t1, b_t2, b_t3 = Buf(), Buf(), Buf()
        yb = sb("p4_yb", [128, D], BF16)
        b_yb = Buf()
        yT = sb("p4_yT", [128, 8, 128], BF16)
        b_yT = Buf()
        junk = sb("p4_junk", [128, D], BF16)
        b_junk = Buf()
        ss = sb("p4_ss", [128, 1], F32)
        rstd = sb("p4_rstd", [128, 1], F32)
        b_ss, b_rstd = Buf(), Buf()
        hx2 = sb("p4_hx2", [128, D], F32)
        b_hx2 = Buf()
        hTf = sb("p4_hTf", [128, 8, 128], F32)
        hxb = [sb(f"p4_hxb{i}", [128, D], BF16) for i in range(2)]
        b_hTf = Buf()
        b_hxb = [Buf(), Buf()]
        pA = ps("p4_pA", [128, D], F32)
        pB = ps("p4_pB", [128, D], F32)
        pC = ps("p4_pC", [128, D], F32)
        pY = ps("p4_pY", [128, 8, 128], BF16)
        pR = ps("p4_pR", [128, 512], F32)
        b_pA, b_pB, b_pC, b_pY, b_pR = Buf(), Buf(), Buf(), Buf(), Buf()
        lg = sb("p4_lg", [128, 36], F32)
        gmx = sb("p4_gmx", [128, 2], F32)
        gmask = sb("p4_gmask", [128, 4], F32)
        gj = sb("p4_gj", [128, 4], F32)
        gsum = sb("p4_gsum", [128, 1], F32)
        elm = sb("p4_elm", [128, 32], F32)
        top8 = sb("p4_top8", [128, 8], F32)
        wts = sb("p4_wts", [128, 4], F32)
        rt = [sb(f"p4_rt{i}", [128, 66], F32) for i in range(2)]
        b_r = Buf()
        b_rt = [Buf(), Buf()]
        tiles = list(tiles)

        def issue_loads(ti):
            if ti >= len(tiles):
                return
            t = tiles[ti]
            j = ti % NB
            u0 = t * 128
            P.dma("sync", oaT[j][:], S["OAT"][:, :, u0:u0 + 128].rearrange("h p t -> p h t"), writes=[b_oa[j]])
            P.dma("sync", obT[j][:], S["OBT"][:, :, u0:u0 + 128].rearrange("h p t -> p h t"), writes=[b_ob[j]])
            P.dma("sync", gat[j][:], S["GA"][u0:u0 + 128, :], writes=[b_ga[j]])
            P.dma("sync", gbt[j][:], S["GB"][u0:u0 + 128, :], writes=[b_gb[j]])
            P.dma("sync", xt[j][:], I["xall"][u0:u0 + 128, :], writes=[b_xt[j]])

        def stA(ti):
            j = ti % NB
            for c in range(2):
                for kc in range(8):
                    P.op("pe", lambda e, c=c, kc=kc: e.matmul(pA[:, c * 512:(c + 1) * 512], lhsT=oaT[j][:, kc, :], rhs=Wa[:, kc, c * 512:(c + 1) * 512],
                                                             start=(kc == 0), stop=(kc == 7)),
                         reads=[b_oa[j], b_Wa[kc][c]], writes=[b_pA], inc=(kc == 7 and c == 1))
            for c in range(2):
                for kc in range(4):
                    P.op("pe", lambda e, c=c, kc=kc: e.matmul(pB[:, c * 512:(c + 1) * 512], lhsT=obT[j][:, kc, :], rhs=Wbb[:, kc, c * 512:(c + 1) * 512],
                                                             start=(kc == 0), stop=(kc == 3)),
                         reads=[b_ob[j], b_Wb[kc][c]], writes=[b_pB], inc=(kc == 3 and c == 1))

        def stB(ti):
            j = ti % NB
            P.op("dve", lambda e: e.tensor_tensor(out=t1[:], in0=pA[:], in1=gat[j][:], op=ALU.mult), reads=[b_pA, b_ga[j]], writes=[b_t1])
            P.op("dve", lambda e: e.tensor_tensor(out=t2[:], in0=pB[:], in1=gbt[j][:], op=ALU.mult), reads=[b_pB, b_gb[j]], writes=[b_t2])
            P.op("pool", lambda e: e.tensor_tensor(out=yb[:], in0=t1[:], in1=t2[:], op=ALU.add), reads=[b_t1, b_t2], writes=[b_yb])

        def stC(ti):
            for kc in range(8):
                P.op("pe", lambda e, kc=kc: e.transpose(pY[:, kc, :], yb[:, kc * 128:(kc + 1) * 128], K.identb[:]),
                     reads=[b_yb, K.b_identb], writes=[b_pY], inc=(kc == 7))
            P.op("act", lambda e: e.copy(out=yT[:], in_=pY[:]), reads=[b_pY], writes=[b_yT])
            for c in range(2):
                for kc in range(8):
                    P.op("pe", lambda e, c=c, kc=kc: e.matmul(pC[:, c * 512:(c + 1) * 512], lhsT=yT[:, kc, :], rhs=Wo[:, kc, c * 512:(c + 1) * 512],
                                                             start=(kc == 0), stop=(kc == 7)),
                         reads=[b_yT, b_Wo[kc][c]], writes=[b_pC], inc=(kc == 7 and c == 1))

        def stD(ti):
            t = tiles[ti]
            j, d, u0 = ti % NB, ti % 2, t * 128
            P.op("dve", lambda e: e.tensor_tensor(out=t3[:], in0=pC[:], in1=ga1[:], op=ALU.mult), reads=[b_pC, b_ga1], writes=[b_t3])
            P.op("dve", lambda e: e.tensor_tensor(out=xm[d][:], in0=t3[:], in1=xt[j][:], op=ALU.add), reads=[b_t3, b_xt[j]], writes=[b_xm[d]])
            P.dma("sync", S["XMID"][u0:u0 + 128, :], xm[d][:], reads=[b_xm[d]])
            P.op("act", lambda e: e.activation(out=junk[:], in_=xm[d][:], func=AF.Square, accum_out=ss[:]),
                 reads=[b_xm[d]], writes=[b_junk, b_ss])
            P.op("dve", lambda e: e.tensor_scalar(out=ss[:], in0=ss[:], scalar1=1.0 / D, scalar2=EPS, op0=ALU.mult, op1=ALU.add),
                 reads=[b_ss], writes=[b_ss])
            P.op("pool", lambda e: e.tensor_tensor(out=rstd[:], in0=ss[:], in1=nhalf[:], op=ALU.pow), reads=[b_ss, b_c], writes=[b_rstd])
            P.op("dve", lambda e: e.scalar_tensor_tensor(out=hx2[:], in0=xm[d][:], scalar=rstd[:, 0:1], in1=g2[:], op0=ALU.mult, op1=ALU.mult),
                 reads=[b_xm[d], b_rstd, b_g2], writes=[b_hx2])
            P.op("dve", lambda e: e.tensor_tensor(out=hx2[:], in0=hx2[:], in1=sh2[:], op=ALU.add), reads=[b_hx2, b_sh2], writes=[b_hx2])

        def stE(ti):
            t = tiles[ti]
            d, u0 = ti % 2, t * 128
            pCv = pC[:].rearrange("p (a b) -> p a b", b=128)
            for kc in range(8):
                P.op("pe", lambda e, kc=kc: e.transpose(pCv[:, kc, :], hx2[:, kc * 128:(kc + 1) * 128], K.ident[:]),
                     reads=[b_hx2, K.b_ident], writes=[b_pC], inc=(kc == 7))
            P.op("act", lambda e: e.copy(out=hTf[:], in_=pCv), reads=[b_pC], writes=[b_hTf])
            P.op("act", lambda e: e.copy(out=hxb[d][:], in_=hx2[:]), reads=[b_hx2], writes=[b_hxb[d]])
            P.dma("sync", S["HX2"][u0:u0 + 128, :], hxb[d][:], reads=[b_hxb[d]])
            for kc in range(8):
                P.op("pe", lambda e, kc=kc: e.matmul(pR[:, 0:36], lhsT=hTf[:, kc, :], rhs=Wr[:, kc, :], start=(kc == 0), stop=(kc == 7)),
                     reads=[b_hTf, b_Wr], writes=[b_pR], inc=(kc == 7))

        def stF(ti):
            t = tiles[ti]
            d, u0 = ti % 2, t * 128
            R_ = [b_r]
            P.op("dve", lambda e: e.tensor_tensor(out=lg[:], in0=pR[:, 0:36], in1=brt[:], op=ALU.add), reads=[b_pR, b_brt], writes=R_)
            P.op("dve", lambda e: e.tensor_reduce(out=gmx[:, 0:1], in_=lg[:, 0:4], axis=AX.X, op=ALU.max), reads=R_, writes=R_)
            P.op("dve", lambda e: e.tensor_scalar(out=gmx[:, 1:2], in0=gmx[:, 0:1], scalar1=-1.0, scalar2=None, op0=ALU.mult), reads=R_, writes=R_)
            P.op("dve", lambda e: e.tensor_scalar(out=gmask[:], in0=lg[:, 0:4], scalar1=gmx[:, 0:1], scalar2=None, op0=ALU.is_equal), reads=R_, writes=R_)
            P.op("act", lambda e: e.activation(out=gj[:], in_=lg[:, 0:4], func=AF.Exp, bias=gmx[:, 1:2], scale=1.0, accum_out=gsum[:]), reads=R_, writes=R_)
            P.op("dve", lambda e: e.reciprocal(out=gsum[:], in_=gsum[:]), reads=R_, writes=R_)
            P.op("dve", lambda e: e.tensor_scalar(out=gmask[:], in0=gmask[:], scalar1=1.0, scalar2=BIG, op0=ALU.subtract, op1=ALU.mult), reads=R_, writes=R_)
            P.op("dve", lambda e: e.tensor_tensor(out=elm[:].rearrange("p (g x) -> p g x", x=8), in0=lg[:, 4:36].rearrange("p (g x) -> p g x", x=8),
                                                  in1=gmask[:].unsqueeze(2).to_broadcast([128, 4, 8]), op=ALU.add), reads=R_, writes=R_)
            P.op("dve", lambda e: e.max(out=top8[:], in_=elm[:]), reads=R_, writes=R_)
            Rd = [b_r, b_rt[d]]
            P.op("dve", lambda e: e.tensor_scalar(out=rt[d][:, 0:32], in0=elm[:], scalar1=top8[:, 0:1], scalar2=None, op0=ALU.is_equal), reads=R_, writes=Rd)
            P.op("dve", lambda e: e.tensor_scalar(out=rt[d][:, 32:64], in0=elm[:], scalar1=top8[:, 1:2], scalar2=None, op0=ALU.is_equal), reads=R_, writes=Rd)
            P.op("dve", lambda e: e.tensor_tensor(out=wts[:, 0:1], in0=top8[:, 1:2], in1=top8[:, 0:1], op=ALU.subtract), reads=R_, writes=R_)
            P.op("act", lambda e: e.activation(out=wts[:, 1:2], in_=wts[:, 0:1], func=AF.Exp), reads=R_, writes=R_)
            P.op("dve", lambda e: e.tensor_scalar(out=wts[:, 2:3], in0=wts[:, 1:2], scalar1=1.0, scalar2=None, op0=ALU.add), reads=R_, writes=R_)
            P.op("dve", lambda e: e.reciprocal(out=wts[:, 2:3], in_=wts[:, 2:3]), reads=R_, writes=R_)
            P.op("dve", lambda e: e.tensor_tensor(out=rt[d][:, 64:65], in0=wts[:, 2:3], in1=gsum[:], op=ALU.mult), reads=R_, writes=Rd)
            P.op("dve", lambda e: e.tensor_tensor(out=rt[d][:, 65:66], in0=rt[d][:, 64:65], in1=wts[:, 1:2], op=ALU.mult), reads=Rd, writes=Rd)
            P.dma("sync", S["ROUT"][u0:u0 + 128, :], rt[d][:], reads=[b_rt[d]])

        n = len(tiles)
        issue_loads(0)
        issue_loads(1)
        for ti in range(n + 1):
            issue_loads(ti + 2)
            if ti < n:
                stA(ti)
            if ti >= 1:
                stD(ti - 1)
                stE(ti - 1)
            if ti < n:
                stB(ti)
                stC(ti)
            if ti >= 1:
                stF(ti - 1)
        P.wait_all_dma("sync")


def phase4s(K, nslot=NSLOT, stop=99):
    nc, P, I, S = K.nc, K.P, K.I, K.S
    IOA = bass.IndirectOffsetOnAxis
    NW = 3
    with contextlib.ExitStack() as st:
        sb = lambda n, s, d: st.enter_context(nc.sbuf_tensor(n, s, d))
        ps = lambda n, s, d: st.enter_context(nc.psum_tensor(n, s, d))
        RT = sb("r_RT", [128, 32, 66], F32)
        b_RT = [Buf() for _ in range(4)]
        rv = S["ROUT"].rearrange("(j p) c -> p j c", p=128)
        for q in range(4):
            P.dma("sync", RT[:, q * 8:(q + 1) * 8, :], rv[:, q * 8:(q + 1) * 8, :], writes=[b_RT[q]])
        ga2 = sb("r_ga2", [128, D], F32)
        b_ga2 = Buf()
        load_bcast(K, P, ga2[:], S["M"][0:1, 5120:6144], b_ga2)
        b_c = Buf()
        onesb = sb("r_onesb", [128, 128], BF16)
        trif = sb("r_trif", [128, 128], F32)
        trib = sb("r_trib", [128, 128], BF16)
        thr32i = sb("r_thr32i", [128, 32], I32)
        thr96i = sb("r_thr96i", [128, NSLOT], I32)
        pidxi = sb("r_pidxi", [128, 1], I32)
        thr32 = sb("r_thr32", [128, 32], F32)
        thr96 = sb("r_thr96", [128, NSLOT], F32)
        pidx = sb("r_pidx", [128, 1], F32)
        tokid = sb("r_tokid", [128, 32], I32)
        zt = sb("r_zt", [NSLOT, 128], I32)
        P.op("pool", lambda e: e.memset(onesb[:], 1.0), writes=[b_c])
        trii = sb("r_trii", [128, 128], I32)
        P.op("pool", lambda e: e.iota(trii[:], pattern=[[1, 128]], base=0, channel_multiplier=-1), writes=[b_c])
        P.op("pool", lambda e: e.tensor_copy(trif[:], trii[:]), reads=[b_c], writes=[b_c])
        P.op("pool", lambda e: e.tensor_scalar(out=trib[:], in0=trif[:], scalar1=0.0, scalar2=None, op0=ALU.is_gt), reads=[b_c], writes=[b_c])
        P.op("pool", lambda e: e.iota(thr32i[:], pattern=[[128, 32]], base=0, channel_multiplier=0), writes=[b_c])
        P.op("pool", lambda e: e.iota(thr96i[:], pattern=[[128, NSLOT]], base=0, channel_multiplier=0), writes=[b_c])
        P.op("pool", lambda e: e.iota(pidxi[:], pattern=[[0, 1]], base=0, channel_multiplier=1), writes=[b_c])
        P.op("pool", lambda e: e.iota(tokid[:], pattern=[[128, 32]], base=0, channel_multiplier=1), writes=[b_c])
        P.op("pool", lambda e: e.memset(zt[:], 0), writes=[b_c])
        P.op("pool", lambda e: e.tensor_copy(thr32[:], thr32i[:]), reads=[b_c], writes=[b_c])
        P.op("pool", lambda e: e.tensor_copy(thr96[:], thr96i[:]), reads=[b_c], writes=[b_c])
        P.op("pool", lambda e: e.tensor_copy(pidx[:], pidxi[:]), reads=[b_c], writes=[b_c])
        b_tok0 = Buf()
        P.dma("sync", S["TOK"].rearrange("(a b) o -> a (b o)", b=128), zt[:], reads=[b_c], writes=[b_tok0])
        P.flush()

        maskb = sb("r_maskb", [128, 32, 32], BF16)
        b_mask = Buf()
        P.op("dve", lambda e: e.tensor_tensor(out=maskb[:], in0=RT[:, :, 0:32], in1=RT[:, :, 32:64], op=ALU.add),
             reads=b_RT, writes=[b_mask])
        pW = ps("r_pW", [128, 32, 32], F32)
        pTot = ps("r_pTot", [128, 32, 32], F32)
        b_pW, b_pTot = Buf(), Buf()
        mflat = maskb[:].rearrange("p j e -> p (j e)")
        ptflat = pTot[:].rearrange("p j e -> p (j e)")
        for c in range(2):
            P.op("pe", lambda e, c=c: e.matmul(ptflat[:, c * 512:(c + 1) * 512], lhsT=onesb[:], rhs=mflat[:, c * 512:(c + 1) * 512],
                                               start=True, stop=True), reads=[b_c, b_mask], writes=[b_pTot], inc=(c == 1))
        for j in range(32):
            P.op("pe", lambda e, j=j: e.matmul(pW[:, j, :], lhsT=trib[:], rhs=maskb[:, j, :], start=True, stop=True),
                 reads=[b_c, b_mask], writes=[b_pW], inc=(j == 31))
        cs = [sb(f"r_cs{i}", [128, 32, 32], F32) for i in range(2)]
        b_cs = [Buf(), Buf()]
        P.op("act", lambda e: e.copy(out=cs[0][:], in_=pTot[:]), reads=[b_pTot], writes=[b_cs[0]])
        a = 0
        for sft in (1, 2, 4, 8, 16):
            P.op("dve", lambda e, a=a, sft=sft: e.tensor_tensor(out=cs[1 - a][:, sft:, :], in0=cs[a][:, sft:, :], in1=cs[a][:, :32 - sft, :], op=ALU.add),
                 reads=[b_cs[a]], writes=[b_cs[1 - a]])
            P.op("dve", lambda e, a=a, sft=sft: e.tensor_copy(cs[1 - a][:, :sft, :], cs[a][:, :sft, :]), reads=[b_cs[a]], writes=[b_cs[1 - a]])
            a = 1 - a
        incl, b_incl = cs[a], b_cs[a]
        R, b_R = cs[1 - a], b_cs[1 - a]
        G = sb("r_G", [128, 32, 32], F32)
        b_G = Buf()
        sm = Buf()
        ce = sb("r_ce", [128, 32], F32)
        pad = [sb(f"r_pad{i}", [128, 32], F32) for i in range(2)]
        base = sb("r_base", [128, 32], F32)
        P.op("dve", lambda e: e.tensor_tensor(out=R[:], in0=incl[:], in1=pTot[:], op=ALU.subtract), reads=[b_incl, b_pTot], writes=[b_R])
        P.op("dve", lambda e: e.tensor_tensor(out=R[:], in0=R[:], in1=pW[:], op=ALU.add), reads=[b_R, b_pW], writes=[b_R])
        P.op("dve", lambda e: e.tensor_tensor(out=G[:], in0=incl[:, 31, :].unsqueeze(2).to_broadcast([128, 32, 32]),
                                              in1=thr32[:].unsqueeze(1).to_broadcast([128, 32, 32]), op=ALU.is_gt),
             reads=[b_incl, b_c], writes=[b_G])
        P.op("dve", lambda e: e.tensor_reduce(out=ce[:], in_=G[:], axis=AX.X, op=ALU.add), reads=[b_G], writes=[sm])
        P.op("dve", lambda e: e.tensor_scalar(out=pad[0][:], in0=ce[:], scalar1=128.0, scalar2=None, op0=ALU.mult), reads=[sm], writes=[sm])
        P.op("dve", lambda e: e.tensor_copy(base[:], pad[0][:]), reads=[sm], writes=[sm])
        a2 = 0
        for sft in (1, 2, 4, 8, 16):
            P.op("dve", lambda e, a2=a2, sft=sft: e.tensor_tensor(out=pad[1 - a2][:, sft:], in0=pad[a2][:, sft:], in1=pad[a2][:, :32 - sft], op=ALU.add),
                 reads=[sm], writes=[sm])
            P.op("dve", lambda e, a2=a2, sft=sft: e.tensor_copy(pad[1 - a2][:, :sft], pad[a2][:, :sft]), reads=[sm], writes=[sm])
            a2 = 1 - a2
        endv = pad[a2]
        P.op("dve", lambda e: e.tensor_tensor(out=base[:], in0=endv[:], in1=base[:], op=ALU.subtract), reads=[sm], writes=[sm])
        P.op("dve", lambda e: e.tensor_tensor(out=R[:], in0=R[:], in1=base[:].unsqueeze(1).to_broadcast([128, 32, 32]), op=ALU.add),
             reads=[b_R, sm], writes=[b_R])
        posf = sb("r_posf", [128, 2, 32], F32)
        posi = sb("r_posi", [128, 64], I32)
        b_pos = Buf()
        for k in range(2):
            P.op("dve", lambda e, k=k: e.tensor_tensor(out=G[:], in0=R[:], in1=RT[:, :, k * 32:(k + 1) * 32], op=ALU.mult),
                 reads=[b_R] + b_RT, writes=[b_G])
            P.op("dve", lambda e, k=k: e.tensor_reduce(out=posf[:, k, :], in_=G[:], axis=AX.X, op=ALU.add), reads=[b_G], writes=[b_pos])
        P.op("dve", lambda e: e.tensor_copy(posi[:], posf[:].rearrange("p k j -> p (k j)")), reads=[b_pos], writes=[b_pos])
        G2 = sb("r_G2", [128, NSLOT, 32], F32)
        eid = sb("r_eid", [128, NSLOT], F32)
        idxf = sb("r_idxf", [128, NSLOT], F32)
        idxw = sb("r_idxw", [128, NSLOT], I32)
        b_idx = Buf()
        P.op("dve", lambda e: e.tensor_tensor(out=G2[:], in0=endv[:].unsqueeze(1).to_broadcast([128, NSLOT, 32]),
                                              in1=thr96[:].unsqueeze(2).to_broadcast([128, NSLOT, 32]), op=ALU.is_le),
             reads=[sm, b_c], writes=[b_idx])
        P.op("dve", lambda e: e.tensor_reduce(out=eid[:], in_=G2[:], axis=AX.X, op=ALU.add), reads=[b_idx], writes=[b_idx])
        P.op("dve", lambda e: e.scalar_tensor_tensor(out=idxf[:], in0=eid[:], scalar=128.0, in1=pidx[:].to_broadcast([128, NSLOT]),
                                                     op0=ALU.mult, op1=ALU.add), reads=[b_idx, b_c], writes=[b_idx])
        P.op("dve", lambda e: e.tensor_copy(idxw[:], idxf[:]), reads=[b_idx], writes=[b_idx])
        if "DBGI" in S:
            P.dma("sync", S["DBGI"][:, 0:64], posi[:], reads=[b_pos])
            P.dma("sync", S["DBGI"][:, 64:64 + NSLOT], idxw[:], reads=[b_idx])
            P.dma("sync", S["DBGF"][:, 0:32], incl[:, 31, :], reads=[b_incl])
            P.dma("sync", S["DBGF"][:, 32:64], endv[:], reads=[sm])
            P.dma("sync", S["DBGF"][:, 64:64 + NSLOT], eid[:], reads=[b_idx])
        if stop <= 1:
            P.wait_all_dma("sync")
            return
        b_sc = [Buf() for _ in range(64)]
        for k in range(2):
            for j in range(32):
                P.idma(lambda e, k=k, j=j: e.indirect_dma_start(out=S["TOK"], out_offset=IOA(ap=posi[:, k * 32 + j:k * 32 + j + 1], axis=0),
                                                                in_=tokid[:, j:j + 1], in_offset=None, bounds_check=None, oob_is_err=False),
                       reads=[b_pos, b_c, b_tok0], writes=[b_sc[k * 32 + j]])
        P.flush()
        tokall = sb("r_tokall", [128, NSLOT], I32)
        b_tok = [Buf() for _ in range(NSLOT)]
        for i in range(nslot):
            P.dma("sync", tokall[:, i:i + 1], S["TOK"][i * 128:(i + 1) * 128, :], writes=[b_tok[i]])

        if "DBGI" in S:
            P.dma("sync", S["DBGI"][:, 64 + 2 * NSLOT:64 + 3 * NSLOT], tokall[:], reads=b_tok)
        if stop <= 2:
            P.wait_all_dma("sync")
            return
        Wg = [sb(f"m_Wg{i}", [128, 8, 512], BF16) for i in range(NW)]
        Wu = [sb(f"m_Wu{i}", [128, 8, 512], BF16) for i in range(NW)]
        Wd = [sb(f"m_Wd{i}", [128, 4, D], BF16) for i in range(NW)]
        b_Wg = [Buf() for _ in range(NW)]
        b_Wu = [Buf() for _ in range(NW)]
        b_Wd = [Buf() for _ in range(NW)]
        hxg = [sb(f"m_hxg{i}", [128, D], BF16) for i in range(NW)]
        b_hxg = [Buf() for _ in range(NW)]
        hT = [sb(f"m_hT{i}", [128, 8, 128], BF16) for i in range(2)]
        b_hT = [Buf(), Buf()]
        sg = [sb(f"m_sg{i}", [128, 512], BF16) for i in range(2)]
        b_sg = [Buf(), Buf()]
        at = [sb(f"m_at{i}", [128, 512], BF16) for i in range(2)]
        b_at = [Buf(), Buf()]
        aT = [sb(f"m_aT{i}", [128, 4, 128], BF16) for i in range(2)]
        b_aT = [Buf(), Buf()]
        ys = [sb(f"m_ys{i}", [128, D], F32) for i in range(2)]
        b_ys = [Buf(), Buf()]
        pTh = ps("m_pTh", [128, 8, 128], BF16)
        pTa = ps("m_pTa", [128, 8, 128], BF16)
        pg = [ptflat[:, i * 512:(i + 1) * 512] for i in range(2)]
        pu = [ps(f"m_pu{i}", [128, 512], F32) for i in range(2)]
        b_pTh, b_pTa = Buf(), Buf()
        b_pg, b_pu = [b_pTot, Buf()], [Buf(), Buf()]
        py = pW
        pyf = pW[:].rearrange("p j e -> p (j e)")
        b_py = b_pW
        regw = nc.alloc_register(mybir.EngineType.Pool, "moe_bc_w")
        bc = {}

        def set_bounds(e):
            e.reg_mov(regw, 32 * 128 - 1)
            bc['w'] = e.snap(regw)
        P.q["pool"].append(set_bounds)

        def issue_gathers(i):
            w = i % NW
            P.idma(lambda e: e.indirect_dma_start(out=hxg[w][:], out_offset=None, in_=S["HX2"], in_offset=IOA(ap=tokall[:, i:i + 1], axis=0),
                                                  bounds_check=None, oob_is_err=False), reads=[b_tok[i]], writes=[b_hxg[w]])
            for Wt, b_Wt, name in ((Wg, b_Wg, "WG16"), (Wu, b_Wu, "WU16"), (Wd, b_Wd, "WD16")):
                P.idma(lambda e, Wt=Wt, name=name: e.indirect_dma_start(out=Wt[w][:].rearrange("p a b -> p (a b)"), out_offset=None, in_=S[name],
                                                                        in_offset=IOA(ap=idxw[:, i:i + 1], axis=0),
                                                                        bounds_check=bc['w'], oob_is_err=False),
                       reads=[b_idx], writes=[b_Wt[w]])

        for i in range(min(NW - 1, nslot)):
            issue_gathers(i)
        for i in range(nslot):
            w, d = i % NW, i % 2
            if i + NW - 1 < nslot:
                issue_gathers(i + NW - 1)
            for kc in range(8):
                P.op("pe", lambda e, kc=kc, w=w: e.transpose(pTh[:, kc, :], hxg[w][:, kc * 128:(kc + 1) * 128], K.identb[:]),
                     reads=[b_hxg[w], K.b_identb], writes=[b_pTh], inc=(kc == 7))
            P.op("act", lambda e, d=d: e.copy(out=hT[d][:], in_=pTh[:]), reads=[b_pTh], writes=[b_hT[d]])
            for kc in range(8):
                P.op("pe", lambda e, kc=kc, w=w, d=d: e.matmul(pg[d], lhsT=hT[d][:, kc, :], rhs=Wg[w][:, kc, :], start=(kc == 0), stop=(kc == 7)),
                     reads=[b_hT[d], b_Wg[w]], writes=[b_pg[d]], inc=(kc == 7))
            for kc in range(8):
                P.op("pe", lambda e, kc=kc, w=w, d=d: e.matmul(pu[d][:], lhsT=hT[d][:, kc, :], rhs=Wu[w][:, kc, :], start=(kc == 0), stop=(kc == 7)),
                     reads=[b_hT[d], b_Wu[w]], writes=[b_pu[d]], inc=(kc == 7))
            P.op("act", lambda e, d=d: e.activation(out=sg[d][:], in_=pg[d], func=AF.Silu), reads=[b_pg[d]], writes=[b_sg[d]])
            P.op("dve", lambda e, d=d: e.tensor_tensor(out=at[d][:], in0=sg[d][:], in1=pu[d][:], op=ALU.mult), reads=[b_sg[d], b_pu[d]], writes=[b_at[d]])
            for hc in range(4):
                P.op("pe", lambda e, hc=hc, d=d: e.transpose(pTa[:, hc, :], at[d][:, hc * 128:(hc + 1) * 128], K.identb[:]),
                     reads=[b_at[d], K.b_identb], writes=[b_pTa], inc=(hc == 3))
            P.op("act", lambda e, d=d: e.copy(out=aT[d][:], in_=pTa[:, 0:4, :]), reads=[b_pTa], writes=[b_aT[d]])
            for c in range(2):
                for hc in range(4):
                    P.op("pe", lambda e, c=c, hc=hc, w=w, d=d: e.matmul(pyf[:, c * 512:(c + 1) * 512], lhsT=aT[d][:, hc, :], rhs=Wd[w][:, hc, c * 512:(c + 1) * 512],
                                                                       start=(hc == 0), stop=(hc == 3)),
                         reads=[b_aT[d], b_Wd[w]], writes=[b_py], inc=(hc == 3 and c == 1))
            P.op("dve", lambda e, d=d: e.tensor_copy(ys[d][:], pyf), reads=[b_py], writes=[b_ys[d]])
            P.dma("sync", S["Y"][i * 128:(i + 1) * 128, :], ys[d][:], reads=[b_ys[d]])
        P.flush()
        if stop <= 3:
            return

        xm = [sb(f"c_xm{i}", [128, D], F32) for i in range(2)]
        y0 = [sb(f"c_y0{i}", [128, D], F32) for i in range(2)]
        y1 = [sb(f"c_y1{i}", [128, D], F32) for i in range(2)]
        tt = [sb(f"c_t{i}", [128, D], F32) for i in range(2)]
        ot = [sb(f"c_o{i}", [128, D], F32) for i in range(2)]
        b_xm, b_y0, b_y1, b_tt, b_ot = [[Buf(), Buf()] for _ in range(5)]

        def issue_c(j):
            d = j % 2
            P.dma("sync", xm[d][:], S["XMID"][j * 128:(j + 1) * 128, :], writes=[b_xm[d]])
            P.idma(lambda e: e.indirect_dma_start(out=y0[d][:], out_offset=None, in_=S["Y"], in_offset=IOA(ap=posi[:, j:j + 1], axis=0),
                                                  bounds_check=None, oob_is_err=False), reads=[b_pos], writes=[b_y0[d]])
            P.idma(lambda e: e.indirect_dma_start(out=y1[d][:], out_offset=None, in_=S["Y"], in_offset=IOA(ap=posi[:, 32 + j:33 + j], axis=0),
                                                  bounds_check=None, oob_is_err=False), reads=[b_pos], writes=[b_y1[d]])

        issue_c(0)
        for j in range(32):
            d = j % 2
            if j + 1 < 32:
                issue_c(j + 1)
            P.op("dve", lambda e, j=j, d=d: e.tensor_scalar(out=tt[d][:], in0=y0[d][:], scalar1=RT[:, j, 64:65], scalar2=None, op0=ALU.mult),
                 reads=[b_y0[d]] + b_RT, writes=[b_tt[d]])
            P.op("dve", lambda e, j=j, d=d: e.scalar_tensor_tensor(out=tt[d][:], in0=y1[d][:], scalar=RT[:, j, 65:66], in1=tt[d][:], op0=ALU.mult, op1=ALU.add),
                 reads=[b_y1[d], b_tt[d]] + b_RT, writes=[b_tt[d]])
            P.op("dve", lambda e, d=d: e.tensor_tensor(out=tt[d][:], in0=tt[d][:], in1=ga2[:], op=ALU.mult), reads=[b_tt[d], b_ga2], writes=[b_tt[d]])
            P.op("dve", lambda e, d=d: e.tensor_tensor(out=ot[d][:], in0=tt[d][:], in1=xm[d][:], op=ALU.add), reads=[b_tt[d], b_xm[d]], writes=[b_ot[d]])
            P.dma("sync", K.out[j * 128:(j + 1) * 128, :], ot[d][:], reads=[b_ot[d]])
        P.wait_all_dma("sync")


def _rope_tables(half):
    u = np.arange(NT)
    l = u // 64
    w = u % 64
    r = l if half == 0 else 127 - l
    pos = np.stack([r, w], axis=-1).astype(np.float32)
    inv_freq = (np.float32(10000.0) ** (-np.arange(16, dtype=np.float32) / np.float32(16))).astype(np.float32)
    ang = (pos[:, :, None] * inv_freq).astype(np.float32)
    cos = np.cos(ang).astype(np.float32)
    sin = np.sin(ang).astype(np.float32)
    C = np.ones((NKA, 2, 2, 16), np.float32)
    Sg = np.zeros((NKA, 2, 2, 16), np.float32)
    C[:NT, :, 0, :] = cos
    C[:NT, :, 1, :] = cos
    Sg[:NT, :, 0, :] = -sin
    Sg[:NT, :, 1, :] = sin
    return C.reshape(NKA, 64), Sg.reshape(NKA, 64)


def _na_bias_tables(rel_bias, half):
    pad = np.concatenate([rel_bias.reshape(8, -1), np.full((8, 1), NEG, np.float32)], axis=1)
    out = np.empty((2, 8, 8, 128, 512), np.float32)
    for v, slot in enumerate((0, 3)):
        R = 8 * slot
        KR0 = min(max(R - 4, 0), 112)
        ql = R + np.arange(8)
        kl = KR0 + np.arange(16)
        g = (lambda a: a) if half == 0 else (lambda a: 127 - a)
        qg = g(ql)[None, None, :, None]
        kg = g(kl)[:, None, None, None]
        kc = np.arange(64)[None, :, None, None]
        qw = np.arange(64)[None, None, None, :]
        r0 = np.clip(qg - 4, 0, 120)
        c0 = np.clip(qw - 8, 0, 48)
        valid = (kg >= r0) & (kg <= r0 + 7) & (kc >= c0) & (kc <= c0 + 15)
        idx = (kg - qg + 7) * 31 + (kc - qw + 15)
        idx = np.where(valid, idx, 465)
        idx = np.broadcast_to(idx, (16, 64, 8, 64)).reshape(8, 128, 512)
        out[v] = pad[:, idx]
    return out


def prep_core_inputs(core, inp):
    b, half = core // 2, core % 2
    xb = np.asarray(inp["x"][b])
    if half == 1:
        xb = xb.reshape(128, 64, D)[::-1].reshape(NT, D)
    xall = np.ascontiguousarray(np.concatenate([xb, np.asarray(inp["ctx"][b])], axis=0))
    C, Sg = _rope_tables(half)
    m = {
        "xall": xall,
        "cc": np.ascontiguousarray(np.stack([np.asarray(inp["c"][b]), np.asarray(inp["c_ctx"])], axis=0)),
        "ropec": C, "ropes": Sg,
        "w_ada": np.asarray(inp["w_ada"][0]), "b_ada": np.asarray(inp["b_ada"][0]).reshape(1, -1),
        "norm1_w": np.asarray(inp["norm1_w"][0]).reshape(1, -1), "norm2_w": np.asarray(inp["norm2_w"][0]).reshape(1, -1),
        "w_in": np.asarray(inp["w_in"][0]),
        "subln_a": np.asarray(inp["subln_a"][0]).reshape(1, -1),
        "nabias": _na_bias_tables(np.asarray(inp["na_rel_bias"][0]), half),
        "w_branch_a": np.asarray(inp["w_branch_a"][0]), "w_branch_b": np.asarray(inp["w_branch_b"][0]),
        "w_out": np.asarray(inp["w_out"][0]),
        "w_router": np.ascontiguousarray(np.concatenate(
            [np.asarray(inp["w_router_group"][0])] + [np.asarray(inp["w_router_expert"][0][g]) for g in range(4)], axis=1)),
        "b_router": np.concatenate([np.asarray(inp["b_router_group"][0]).reshape(-1),
                                    np.asarray(inp["b_router_expert"][0]).reshape(-1)]).reshape(1, 36),
        "w_expert_gate": np.asarray(inp["w_expert_gate"][0]), "w_expert_up": np.asarray(inp["w_expert_up"][0]),
        "w_expert_down": np.asarray(inp["w_expert_down"][0]),
    }
    for n in ("q_norm_a", "k_norm_a", "lambda_q1", "lambda_k1", "lambda_q2", "lambda_k2", "q_norm_b", "k_norm_b"):
        m[n] = np.asarray(inp[n][0]).reshape(1, -1)
    return {k: np.ascontiguousarray(v, dtype=np.float32) for k, v in m.items()}


def kernel(**inputs):
    nc, _ = build()
    in_maps = [prep_core_inputs(c, inputs) for c in range(8)]
    res = run_bass_kernel_spmd(nc, in_maps, core_ids=list(range(8)))
    outp = np.empty((4, NT, D), np.float32)
    for c in range(8):
        b, half = c // 2, c % 2
        o = np.asarray(res.results[c]["out"]).reshape(64, 64, D)
        if half == 0:
            outp[b, :NOWN] = o.reshape(NOWN, D)
        else:
            outp[b, NOWN:] = o[::-1].reshape(NOWN, D)
    return outp
```
